# Optimizing a Trainium2 kernel written in Bass

```python
import jax
import jax.numpy as jnp
from jax import lax
import numpy as np

D_MODEL = 1024
BATCH = 8
SEQ = 4096
DEPTH = 2

GRID_W = 64
CTX_LEN = 256
HEAD_DIM = 64
NA_HEADS = 8
NA_WIDTH = NA_HEADS * HEAD_DIM
NA_WIN_ROWS = 8
NA_WIN_COLS = 16
NA_QBLOCK_COLS = 16
NA_KBLOCK_COLS = 2 * NA_WIN_COLS
LRU_WIDTH = D_MODEL // 2
LRU_BLOCKS = 8
LRU_BLOCK = LRU_WIDTH // LRU_BLOCKS
LRU_CONV_W = 4
LRU_C = 8.0
MIX_WIDTH = NA_WIDTH + LRU_WIDTH
MIX_IN_WIDTH = 3 * NA_WIDTH + 2 * LRU_WIDTH
FNET_GROUPS = 4
N_EXPERTS = 16
N_EXPERT_GROUPS = 4
EXPERTS_PER_GROUP = N_EXPERTS // N_EXPERT_GROUPS
TOP_K = 2
D_FF_EXPERT = 512
N_EVEN_LAYERS = (DEPTH + 1) // 2
N_ODD_LAYERS = DEPTH // 2
RMS_EPS = 1e-6
MASK_VALUE = -1e30

kernel_name = "hybrid_na_rglru_fnet_grouped_moe"


def rmsnorm(x, gain):
    xf = x.astype(jnp.float32)
    y = xf * lax.rsqrt(jnp.mean(xf * xf, axis=-1, keepdims=True) + RMS_EPS)
    return (y * gain.astype(jnp.float32)).astype(x.dtype)


def modulate(h, shift, scale):
    return h * (1 + scale) + shift


def na_tables(rows):
    kr = min(NA_WIN_ROWS, rows)
    r = np.arange(rows)
    row_start = np.clip(r - kr // 2, 0, rows - kr)
    key_rows = row_start[:, None] + np.arange(kr)
    ncb = GRID_W // NA_QBLOCK_COLS
    c0 = np.arange(ncb) * NA_QBLOCK_COLS
    kb = np.clip(c0 - NA_WIN_COLS // 2, 0, GRID_W - NA_KBLOCK_COLS)
    key_cols = kb[:, None] + np.arange(NA_KBLOCK_COLS)
    q_cols = c0[:, None] + np.arange(NA_QBLOCK_COLS)
    col_start = np.clip(q_cols - NA_WIN_COLS // 2, 0, GRID_W - NA_WIN_COLS)
    idx = (key_rows[:, None, :, None] * GRID_W + key_cols[None, :, None, :]).reshape(rows, ncb, kr * NA_KBLOCK_COLS)
    kc = key_cols[:, None, :]
    valid = (kc >= col_start[..., None]) & (kc < col_start[..., None] + NA_WIN_COLS)
    dr = key_rows - r[:, None] + (NA_WIN_ROWS - 1)
    dc = np.clip(kc - q_cols[..., None], 1 - NA_WIN_COLS, NA_WIN_COLS - 1) + (NA_WIN_COLS - 1)
    return idx.astype(np.int32), valid, dr, dc, kr


def neighbourhood_attention(q, k, v, k_ctx, v_ctx, rpb):
    B, N, H, dh = q.shape
    rows = N // GRID_W
    ncb = GRID_W // NA_QBLOCK_COLS
    idx, valid, dr, dc, kr = na_tables(rows)
    n_keys = kr * NA_KBLOCK_COLS
    bias = rpb.astype(jnp.float32)[:, dr[:, None, None, :, None], dc[None, :, :, None, :]]
    bias = jnp.where(valid[None, None, :, :, None, :], bias, MASK_VALUE)
    bias = bias.transpose(1, 0, 2, 3, 4, 5).reshape(rows, H, ncb, NA_QBLOCK_COLS, n_keys)
    q_rows = q.reshape(B, rows, ncb, NA_QBLOCK_COLS, H, dh).transpose(1, 0, 2, 3, 4, 5)

    def row_block(args):
        q_r, idx_r, bias_r = args
        k_g = jnp.take(k, idx_r, axis=1)
        v_g = jnp.take(v, idx_r, axis=1)
        s_win = jnp.einsum("bcqhd,bckhd->bhcqk", q_r, k_g, preferred_element_type=jnp.float32) + bias_r[None]
        s_ctx = jnp.einsum("bcqhd,bjhd->bhcqj", q_r, k_ctx, preferred_element_type=jnp.float32)
        p = jax.nn.softmax(jnp.concatenate([s_win, s_ctx], axis=-1), axis=-1).astype(v.dtype)
        return (jnp.einsum("bhcqk,bckhd->bcqhd", p[..., :n_keys], v_g)
                + jnp.einsum("bhcqj,bjhd->bcqhd", p[..., n_keys:], v_ctx))

    o = lax.map(row_block, (q_rows, jnp.asarray(idx), bias))
    return o.transpose(1, 0, 2, 3, 4, 5).reshape(B, N, H * dh)


def centred_depthwise_conv(x, w, b):
    T = x.shape[1]
    lo = LRU_CONV_W // 2
    xp = jnp.pad(x, ((0, 0), (lo, LRU_CONV_W - 1 - lo), (0, 0)))
    y = b
    for tap in range(LRU_CONV_W):
        y = y + w[tap] * xp[:, tap:tap + T]
    return y


def rglru_coeffs(xc, w_r, b_r, w_i, b_i, lam):
    B, T, W = xc.shape
    xb = xc.reshape(B, T, LRU_BLOCKS, LRU_BLOCK)
    r = jax.nn.sigmoid(jnp.einsum("bthi,hij->bthj", xb, w_r.astype(jnp.float32)).reshape(B, T, W) + b_r.astype(jnp.float32))
    i = jax.nn.sigmoid(jnp.einsum("bthi,hij->bthj", xb, w_i.astype(jnp.float32)).reshape(B, T, W) + b_i.astype(jnp.float32))
    log_a = -LRU_C * r * jax.nn.softplus(-lam.astype(jnp.float32))
    return jnp.exp(log_a), jnp.sqrt(-jnp.expm1(2.0 * log_a)) * (i * xc)


def linear_scan(a, b):
    def combine(left, right):
        return left[0] * right[0], right[0] * left[1] + right[1]
    return lax.associative_scan(combine, (a, b), axis=1)


def bidirectional_rglru(x_lat, x_ctx, w_r, b_r, w_i, b_i, lam):
    x_lat = x_lat.astype(jnp.float32)
    x_ctx = x_ctx.astype(jnp.float32)
    outs = []
    for d in range(2):
        a_c, b_c = rglru_coeffs(x_ctx, w_r[d], b_r[d], w_i[d], b_i[d], lam[d])
        a_l, b_l = rglru_coeffs(x_lat, w_r[d], b_r[d], w_i[d], b_i[d], lam[d])
        if d == 1:
            a_c, b_c, a_l, b_l = (jnp.flip(t, axis=1) for t in (a_c, b_c, a_l, b_l))
        h0 = linear_scan(a_c, b_c)[1][:, -1]
        a_prefix, h_l = linear_scan(a_l, b_l)
        h = h_l + a_prefix * h0[:, None, :]
        if d == 1:
            h = jnp.flip(h, axis=1)
        outs.append(h)
    return outs[0] + outs[1]


def grouped_moe(h, router_w, router_bias, w_gate, w_up, w_down):
    B, T, D = h.shape
    scores = jax.nn.sigmoid(jnp.einsum("btd,de->bte", h, router_w, preferred_element_type=jnp.float32))
    sel = scores + router_bias.astype(jnp.float32)
    grp = sel.reshape(B, T, N_EXPERT_GROUPS, EXPERTS_PER_GROUP)
    grp_score = lax.top_k(grp, TOP_K)[0].sum(-1)
    best = jnp.argmax(grp_score, axis=-1)
    in_group = (jnp.arange(N_EXPERTS) // EXPERTS_PER_GROUP)[None, None, :] == best[..., None]
    _, idx = lax.top_k(jnp.where(in_group, sel, -jnp.inf), TOP_K)
    w = jnp.take_along_axis(scores, idx, axis=-1)
    w = w / jnp.sum(w, axis=-1, keepdims=True)
    gates = jnp.sum(jax.nn.one_hot(idx, N_EXPERTS, dtype=jnp.float32) * w[..., None], axis=-2).astype(h.dtype)
    out = jnp.zeros_like(h)
    for e in range(N_EXPERTS):
        y = (jax.nn.silu(h @ w_gate[e]) * (h @ w_up[e])) @ w_down[e]
        out = out + gates[..., e:e + 1] * y
    return out


def setup_inputs(seed: int = 0) -> dict:
    key = jax.random.key(seed)
    keys = iter(jax.random.split(key, 32))
    D = D_MODEL

    def nrm(shape, scale):
        return jax.random.normal(next(keys), shape, jnp.float32) * scale

    x = nrm((BATCH, SEQ, D), 1.0)
    c = nrm((BATCH, D), 1.0)
    ctx = nrm((BATCH, CTX_LEN, D), 1.0)
    c_ctx = nrm((D,), 1.0)
    ada_w = nrm((DEPTH, D, 6 * D), 0.5 * D ** -0.5)
    ada_b = nrm((DEPTH, 6 * D), 0.02)
    norm_mix = 1.0 + nrm((DEPTH, D), 0.02)
    norm_ffn = 1.0 + nrm((DEPTH, D), 0.02)
    mix_w_in = nrm((N_EVEN_LAYERS, D, MIX_IN_WIDTH), D ** -0.5)
    mix_w_out = nrm((N_EVEN_LAYERS, MIX_WIDTH, D), MIX_WIDTH ** -0.5)
    na_q_norm = 1.0 + nrm((N_EVEN_LAYERS, HEAD_DIM), 0.02)
    na_k_norm = 1.0 + nrm((N_EVEN_LAYERS, HEAD_DIM), 0.02)
    na_rpb = nrm((N_EVEN_LAYERS, NA_HEADS, 2 * NA_WIN_ROWS - 1, 2 * NA_WIN_COLS - 1), 0.5)
    lru_conv_w = nrm((N_EVEN_LAYERS, LRU_CONV_W, LRU_WIDTH), LRU_CONV_W ** -0.5)
    lru_conv_b = nrm((N_EVEN_LAYERS, LRU_WIDTH), 0.02)
    lru_gate_r_w = nrm((N_EVEN_LAYERS, 2, LRU_BLOCKS, LRU_BLOCK, LRU_BLOCK), LRU_BLOCK ** -0.5)
    lru_gate_r_b = nrm((N_EVEN_LAYERS, 2, LRU_WIDTH), 0.02)
    lru_gate_i_w = nrm((N_EVEN_LAYERS, 2, LRU_BLOCKS, LRU_BLOCK, LRU_BLOCK), LRU_BLOCK ** -0.5)
    lru_gate_i_b = nrm((N_EVEN_LAYERS, 2, LRU_WIDTH), 0.02)
    a_pow_c = jax.random.uniform(next(keys), (N_EVEN_LAYERS, 2, LRU_WIDTH), jnp.float32, 0.9, 0.999)
    a0 = a_pow_c ** (1.0 / LRU_C)
    lru_lambda = jnp.log(a0) - jnp.log1p(-a0)
    fnet_w_out = nrm((N_ODD_LAYERS, D, D), D ** -0.5)
    router_w = nrm((D, N_EXPERTS), D ** -0.5)
    router_bias = nrm((N_EXPERTS,), 0.01)
    moe_w_gate = nrm((DEPTH, N_EXPERTS, D, D_FF_EXPERT), D ** -0.5)
    moe_w_up = nrm((DEPTH, N_EXPERTS, D, D_FF_EXPERT), D ** -0.5)
    moe_w_down = nrm((DEPTH, N_EXPERTS, D_FF_EXPERT, D), D_FF_EXPERT ** -0.5)
    return {"x": x, "c": c, "ctx": ctx, "c_ctx": c_ctx, "ada_w": ada_w, "ada_b": ada_b,
            "norm_mix": norm_mix, "norm_ffn": norm_ffn, "mix_w_in": mix_w_in, "mix_w_out": mix_w_out,
            "na_q_norm": na_q_norm, "na_k_norm": na_k_norm, "na_rpb": na_rpb,
            "lru_conv_w": lru_conv_w, "lru_conv_b": lru_conv_b,
            "lru_gate_r_w": lru_gate_r_w, "lru_gate_r_b": lru_gate_r_b,
            "lru_gate_i_w": lru_gate_i_w, "lru_gate_i_b": lru_gate_i_b, "lru_lambda": lru_lambda,
            "fnet_w_out": fnet_w_out, "router_w": router_w, "router_bias": router_bias,
            "moe_w_gate": moe_w_gate, "moe_w_up": moe_w_up, "moe_w_down": moe_w_down}


def reference(x, c, ctx, c_ctx, ada_w, ada_b, norm_mix, norm_ffn, mix_w_in, mix_w_out,
              na_q_norm, na_k_norm, na_rpb, lru_conv_w, lru_conv_b,
              lru_gate_r_w, lru_gate_r_b, lru_gate_i_w, lru_gate_i_b, lru_lambda,
              fnet_w_out, router_w, router_bias, moe_w_gate, moe_w_up, moe_w_down):
    B, N, D = x.shape
    silu_c = jax.nn.silu(c)
    silu_c_ctx = jax.nn.silu(c_ctx)

    def heads(t):
        return t.reshape(t.shape[0], t.shape[1], NA_HEADS, HEAD_DIM)

    for layer in range(DEPTH):
        li = layer // 2
        mod = silu_c @ ada_w[layer] + ada_b[layer]
        shift1, scale1, gate1, shift2, scale2, gate2 = jnp.split(mod[:, None, :], 6, axis=-1)
        h = modulate(rmsnorm(x, norm_mix[layer]), shift1, scale1)
        if layer % 2 == 0:
            mod_ctx = silu_c_ctx @ ada_w[layer, :, :2 * D] + ada_b[layer, :2 * D]
            h_ctx = modulate(rmsnorm(ctx, norm_mix[layer]), mod_ctx[:D], mod_ctx[D:])
            w_in = mix_w_in[li]
            q, k, v, xb, gb = jnp.split(h @ w_in, [NA_WIDTH, 2 * NA_WIDTH, 3 * NA_WIDTH, 3 * NA_WIDTH + LRU_WIDTH], axis=-1)
            k_c, v_c, xb_c = jnp.split(h_ctx @ w_in[:, NA_WIDTH:3 * NA_WIDTH + LRU_WIDTH], [NA_WIDTH, 2 * NA_WIDTH], axis=-1)
            q = rmsnorm(heads(q), na_q_norm[li]) * HEAD_DIM ** -0.5
            k = rmsnorm(heads(k), na_k_norm[li])
            k_c = rmsnorm(heads(k_c), na_k_norm[li])
            attn = neighbourhood_attention(q, k, heads(v), k_c, heads(v_c), na_rpb[li])
            xc_lat = centred_depthwise_conv(xb, lru_conv_w[li], lru_conv_b[li])
            xc_ctx = centred_depthwise_conv(xb_c, lru_conv_w[li], lru_conv_b[li])
            y = bidirectional_rglru(xc_lat, xc_ctx, lru_gate_r_w[li], lru_gate_r_b[li],
                                    lru_gate_i_w[li], lru_gate_i_b[li], lru_lambda[li])
            lru = jax.nn.gelu(gb) * y.astype(gb.dtype)
            mix = jnp.concatenate([attn, lru], axis=-1) @ mix_w_out[li]
        else:
            hg = h.astype(jnp.float32).reshape(B, N, FNET_GROUPS, D // FNET_GROUPS)
            f = jnp.fft.fft2(hg, axes=(1, 3), norm="ortho").real.reshape(B, N, D).astype(x.dtype)
            mix = f @ fnet_w_out[li]
        x = x + gate1 * mix
        h2 = modulate(rmsnorm(x, norm_ffn[layer]), shift2, scale2)
        x = x + gate2 * grouped_moe(h2, router_w, router_bias, moe_w_gate[layer], moe_w_up[layer], moe_w_down[layer])
    return x
```

```python
import os
import numpy as np
import ml_dtypes
import concourse.bass as bass
import concourse.mybir as mybir
from concourse.bass_utils import run_bass_kernel_spmd
from contextlib import ExitStack

F32 = mybir.dt.float32
BF16 = mybir.dt.bfloat16
I32 = mybir.dt.int32
AF = mybir.ActivationFunctionType
ALU = mybir.AluOpType

D = 1024
N = 4096
CT = 256
NT = N // 128
NE = 16
DFF = 512
EPS = 1e-6
MASKV = -30000.0
DEBUG = bool(int(os.environ.get("MK_DEBUG", "0")))
LAYERS = int(os.environ.get("MK_LAYERS", "2"))

COMPUTE = ("pe", "act", "dve", "pool")


class Prog:
    def __init__(self, nc, n_dma_sems=12):
        self.nc = nc
        self.ops = []
        self.last_writer = {}
        self.readers = {}
        self.eng_ops = {e: [] for e in COMPUTE + ("sp",)}
        self.n_dma_sems = n_dma_sems
        self.dma_rr = {"sp": 0, "pool": 0, "act": 0}
        self.dma_cnt = {}
        self.dma_last = {}
        self.pending_barrier = {}

    def _deps(self, reads, writes):
        deps = set()
        for r in reads:
            w = self.last_writer.get(r)
            if w is not None:
                deps.add(w)
        for r in writes:
            w = self.last_writer.get(r)
            if w is not None:
                deps.add(w)
            for rd in self.readers.get(r, ()):
                deps.add(rd)
        return deps

    def _commit(self, oid, reads, writes):
        for r in reads:
            self.readers.setdefault(r, []).append(oid)
        for r in writes:
            self.last_writer[r] = oid
            self.readers[r] = []

    def op(self, eng, fn, reads=(), writes=()):
        oid = len(self.ops)
        deps = self._deps(reads, writes)
        pb = self.pending_barrier.pop(eng, None)
        if pb:
            deps |= pb
        o = dict(id=oid, eng=eng, fn=fn, deps=deps, dma=None, pos=len(self.eng_ops[eng]))
        self.ops.append(o)
        self.eng_ops[eng].append(oid)
        self._commit(oid, reads, writes)
        return oid

    def dma(self, q, out, in_, reads=(), writes=(), ind=None, **kw):
        oid = len(self.ops)
        deps = self._deps(reads, writes)
        pb = self.pending_barrier.pop(q, None)
        if pb:
            deps |= pb
        j = self.dma_rr[q]
        self.dma_rr[q] = (j + 1) % self.n_dma_sems
        chan = ("dma", q, j)
        prev = self.dma_last.get(chan)
        if prev is not None:
            deps.add(prev)
        self.dma_cnt[chan] = self.dma_cnt.get(chan, 0) + 16
        self.dma_last[chan] = oid
        o = dict(id=oid, eng=q, fn=None, deps=deps, dma=(out, in_, kw), ind=ind, chan=chan,
                 val=self.dma_cnt[chan], pos=len(self.eng_ops[q]))
        self.ops.append(o)
        self.eng_ops[q].append(oid)
        self._commit(oid, reads, writes)
        return oid

    def barrier(self):
        deps = set()
        for e, lst in self.eng_ops.items():
            if lst:
                deps.add(lst[-1])
        for chan, oid in self.dma_last.items():
            deps.add(oid)
        for e in self.eng_ops:
            self.pending_barrier[e] = set(deps) | self.pending_barrier.get(e, set())

    def emit(self, final_wait_ops=()):
        nc = self.nc
        ops = self.ops

        def chan_of(o):
            return o["chan"] if o["dma"] is not None else ("eng", o["eng"])

        def pos_of(o):
            return o["val"] if o["dma"] is not None else o["pos"] + 1

        clocks = {e: {} for e in self.eng_ops}
        waits = {}
        signals = set()
        snap = {}
        for o in ops:
            e = o["eng"]
            clk = clocks[e]
            need = {}
            for d in o["deps"]:
                p = ops[d]
                c = chan_of(p)
                v = pos_of(p)
                if p["dma"] is None and p["eng"] == e and e in ("pe", "sp"):
                    continue
                if clk.get(c, 0) >= v:
                    continue
                if c not in need or need[c][0] < v:
                    need[c] = (v, d)
            wl = []
            for c, (v, d) in need.items():
                if clk.get(c, 0) >= v:
                    continue
                wl.append(d)
                p = ops[d]
                if p["dma"] is None:
                    signals.add(d)
                for cc, vv in snap[d].items():
                    if clk.get(cc, 0) < vv:
                        clk[cc] = vv
                if clk.get(c, 0) < v:
                    clk[c] = v
            waits[o["id"]] = wl
            snap[o["id"]] = dict(clk)
        for d in final_wait_ops:
            if ops[d]["dma"] is None:
                signals.add(d)
        count_at = {}
        for e in COMPUTE:
            n = 0
            for oid in self.eng_ops[e]:
                if ops[oid]["dma"] is None and oid in signals:
                    n += 1
                    count_at[oid] = n
        self.stats = dict(n_ops=len(ops), n_signals=len(signals),
                          n_waits=sum(len(w) for w in waits.values()),
                          per_eng={e: len(v) for e, v in self.eng_ops.items()})
        es = ExitStack()
        sems = {}
        for e in COMPUTE:
            sems[("eng", e)] = es.enter_context(nc.semaphore(f"s_{e}"))
        for chan in self.dma_cnt:
            sems[chan] = es.enter_context(nc.semaphore(f"s_{chan[1]}_{chan[2]}"))
        block = es.enter_context(nc.Block())

        def wait_val(d):
            p = ops[d]
            return (sems[chan_of(p)], p["val"] if p["dma"] is not None else count_at[d])

        def make(e):
            def body(eng):
                bc_regs = {}
                for oid in self.eng_ops[e]:
                    o = ops[oid]
                    for d in waits[oid]:
                        s, v = wait_val(d)
                        eng.wait_ge(s, v)
                    if o["dma"] is not None:
                        out, in_, kw = o["dma"]
                        if o.get("ind") is not None:
                            oo, io = o["ind"]
                            if isinstance(kw.get("bounds_check"), int):
                                bc = kw["bounds_check"]
                                if bc not in bc_regs:
                                    bc_regs[bc] = eng.to_reg(bc)
                                kw = dict(kw, bounds_check=bc_regs[bc])
                            eng.indirect_dma_start(
                                out=out, out_offset=(None if oo is None else bass.IndirectOffsetOnAxis(ap=oo, axis=0)),
                                in_=in_, in_offset=(None if io is None else bass.IndirectOffsetOnAxis(ap=io, axis=0)),
                                **kw).then_inc(sems[o["chan"]], 16)
                        else:
                            eng.dma_start(out=out, in_=in_, **kw).then_inc(sems[o["chan"]], 16)
                    else:
                        ins = o["fn"](eng)
                        if oid in signals:
                            ins.then_inc(sems[("eng", e)], 1)
                if e == "sp":
                    for d in final_wait_ops:
                        s, v = wait_val(d)
                        eng.wait_ge(s, v)
            return body

        block.tensor(make("pe"))
        block.scalar(make("act"))
        block.vector(make("dve"))
        block.gpsimd(make("pool"))
        block.sync(make("sp"))
        es.close()


class Ring:
    uid = 0

    def __init__(self, es, nc, name, shape, dt, n, psum=False):
        self.name = name
        self.n = n
        self.i = 0
        alloc = nc.psum_tensor if psum else nc.sbuf_tensor
        Ring.uid += 1
        self.bufs = [es.enter_context(alloc(f"{name}{j}_r{Ring.uid}", shape, dt)) for j in range(n)]
        self.name = f"{name}_r{Ring.uid}_"

    def get(self):
        j = self.i % self.n
        self.i += 1
        return self.bufs[j], f"{self.name}{j}"


def build_program():
    nc = bass.Bass("TRN2", target_bir_lowering=False)
    P = Prog(nc, n_dma_sems=12)

    def din(name, shape, dt=F32):
        return nc.dram_tensor(name, list(shape), dt, kind="ExternalInput").ap()

    def dscr(name, shape, dt, dbg=False):
        kind = "ExternalOutput" if (dbg and DEBUG) else "Internal"
        return nc.dram_tensor(name, list(shape), dt, kind=kind).ap()

    x_d = din("x", [N, D])
    c_d = din("c", [8, 128])
    ctx_d = din("ctx", [CT, D])
    cctx_d = din("c_ctx", [8, 128])
    adaw_d = din("ada_w", [2, D, 6 * D])
    adab_d = din("ada_b", [2, 6 * D])
    nmix_d = din("norm_mix", [2, D])
    nffn_d = din("norm_ffn", [2, D])
    win_d = din("mix_w_in", [D, 2560])
    wout_d = din("mix_w_out", [D, D])
    qkg_d = din("qk_gain", [128, 2])
    bt_d = din("bt", [4, 128, 2, 14, 64])
    cw_d = din("lru_cw", [128, 4, 4])
    cb_d = din("lru_cb", [128, 4])
    wbd_d = din("lru_wbd", [128, 16, 128])
    gbias_d = din("lru_gbias", [128, 16])
    lam_d = din("lru_lam", [128, 8])
    fw_d = din("fnet_w_out", [D, D])
    rw_d = din("router_w", [D, NE])
    rb_d = din("router_bias", [1, NE])
    wg_d = din("moe_w_gate", [2 * NE * 128, 8 * DFF])
    wu_d = din("moe_w_up", [2 * NE * 128, 8 * DFF])
    wd_d = din("moe_w_down", [2 * NE * DFF, D])
    cc_d = din("dft_c", [128, 2, 2, 256], BF16)
    dftn_d = din("dft_n", [4, 32, 128, 2, 512], BF16)
    out_d = nc.dram_tensor("out", [N, D], F32, kind="ExternalOutput").ap()

    qT_d = dscr("qT_s", [512, N], BF16, True)
    kT_d = dscr("kT_s", [512, N + CT], BF16, True)
    v_d = dscr("v_s", [N + CT, 512], BF16, True)
    xbT_d = dscr("xbT_s", [512, N + CT], F32, True)
    gbT_d = dscr("gbT_s", [512, N], F32, True)
    cat_d = [dscr("cat0_s", [D, N], BF16, True), dscr("cat1_s", [D, N], BF16, True)]
    xmid_d = dscr("xmid_s", [N, D], F32, True)
    x1_d = dscr("x1_s", [N, D], F32)
    xs_d = dscr("xs_s", [16384, D], BF16)
    y_d = dscr("y_s", [16384, D], F32)

    top = ExitStack()

    def sb(es, name, shape, dt):
        Ring.uid += 1
        return es.enter_context(nc.sbuf_tensor(f"{name}_u{Ring.uid}", list(shape), dt))

    def psm(es, name, shape, dt):
        Ring.uid += 1
        return es.enter_context(nc.psum_tensor(f"{name}_u{Ring.uid}", list(shape), dt))

    ident32 = sb(top, "ident32", [128, 128], F32)
    identbf = sb(top, "identbf", [128, 128], BF16)
    ones32 = sb(top, "ones32", [128, 128], F32)
    onesbf = sb(top, "onesbf", [128, 128], BF16)
    blk32 = sb(top, "blk32", [128, 128], F32)
    blkbf = sb(top, "blkbf", [128, 128], BF16)
    cbT = sb(top, "cbT", [128, 8, 128], F32)
    MOD = sb(top, "MOD", [128, 6 * D], F32)
    rw32 = sb(top, "rw32", [128, 8, NE], F32)
    rbias = sb(top, "rbias", [128, NE], F32)
    cols = sb(top, "cols", [128, 8], F32)

    P.op("pool", lambda e: e.memset(ident32[:], 0.0), writes=["ident32"])
    P.op("pool", lambda e: e.affine_select(ident32[:], ident32[:], [[-1, 128]], ALU.not_equal, 1.0,
                                           base=0, channel_multiplier=1), reads=["ident32"], writes=["ident32"])
    P.op("dve", lambda e: e.tensor_copy(identbf[:], ident32[:]), reads=["ident32"], writes=["identbf"])
    P.op("pool", lambda e: e.memset(ones32[:], 1.0), writes=["ones32"])
    P.op("pool", lambda e: e.memset(onesbf[:], 1.0), writes=["onesbf"])
    P.op("pool", lambda e: e.memset(blk32[:], 0.0), writes=["blk32"])
    P.op("pool", lambda e: e.memset(blk32[0:64, 0:64], 1.0), reads=["blk32"], writes=["blk32"])
    P.op("pool", lambda e: e.memset(blk32[64:128, 64:128], 1.0), reads=["blk32"], writes=["blk32"])
    P.op("dve", lambda e: e.tensor_copy(blkbf[:], blk32[:]), reads=["blk32"], writes=["blkbf"])
    P.op("pool", lambda e: e.memset(cols[:, 0:1], EPS), writes=["cols"])
    P.op("pool", lambda e: e.memset(cols[:, 1:2], 64 * EPS), reads=["cols"], writes=["cols"])
    P.op("pool", lambda e: e.memset(cols[:, 2:3], 1.0), reads=["cols"], writes=["cols"])
    P.dma("sp", rw32[:], rw_d.rearrange("(k p) e -> p k e", p=128), writes=["rw32"])
    P.dma("sp", rbias[:], rb_d.partition_broadcast(128), writes=["rbias"])

    SEC = {"S1": 0, "G1": 1, "GATE1": 2, "S2": 3, "G2": 4, "GATE2": 5}

    def modsec(name):
        j = SEC[name]
        return MOD[:, j * D:(j + 1) * D]

    def make_cb(es, src_d, dst, tag):
        crow = sb(es, f"crow{tag}", [8, 128], F32)
        ccol = sb(es, f"ccol{tag}", [128, 8], F32)
        pt = psm(es, f"cps{tag}", [128, 8], F32)
        P.dma("sp", crow[:], src_d, writes=[f"crow{tag}"])
        P.op("act", lambda e: e.activation(crow[:], crow[:], AF.Silu), reads=[f"crow{tag}"], writes=[f"crow{tag}"])
        P.op("pe", lambda e: e.transpose(pt[:], crow[:], ident32[0:8, 0:8]), reads=[f"crow{tag}", "ident32"],
             writes=[f"cps{tag}"])
        P.op("dve", lambda e: e.tensor_copy(ccol[:], pt[:]), reads=[f"cps{tag}"], writes=[f"ccol{tag}"])
        for k in range(8):
            P.op("dve", lambda e, k=k: e.tensor_scalar(dst[:, k, :], ones32[:], ccol[:, k:k + 1], None, ALU.mult),
                 reads=[f"ccol{tag}", "ones32"], writes=[f"cb{tag}"])

    with ExitStack() as es0:
        make_cb(es0, c_d, cbT, "c")
        P.barrier()

    def phase_mods(l, MODC=None, cxT=None):
        with ExitStack() as es:
            abias = sb(es, "abias", [128, 6 * D], F32)
            gm = sb(es, "gm", [128, D], F32)
            gf = sb(es, "gf", [128, D], F32)
            awr = Ring(es, nc, "aw", [128, 8, 512], F32, 2)
            psr = Ring(es, nc, "modps", [128, 512], F32, 2, psum=True)
            psc = Ring(es, nc, "modpc", [128, 512], F32, 2, psum=True)
            P.dma("sp", abias[:], adab_d[l:l + 1, :].partition_broadcast(128), writes=["abias"])
            P.dma("sp", gm[:], nmix_d[l:l + 1, :].partition_broadcast(128), writes=["gm"])
            P.dma("sp", gf[:], nffn_d[l:l + 1, :].partition_broadcast(128), writes=["gf"])
            for j in range(12):
                aw, ar = awr.get()
                P.dma("sp", aw[:], adaw_d[l][:, j * 512:(j + 1) * 512].rearrange("(k p) n -> p k n", p=128),
                      writes=[ar])
                ps, pr = psr.get()
                for k in range(8):
                    P.op("pe", lambda e, ps=ps, aw=aw, k=k: e.matmul(ps[:], cbT[:, k, :], aw[:, k, :],
                                                                      start=(k == 0), stop=(k == 7)),
                         reads=[ar, "cbc"], writes=[pr])
                P.op("dve", lambda e, ps=ps, j=j: e.tensor_tensor(MOD[:, j * 512:(j + 1) * 512], ps[:],
                                                                   abias[:, j * 512:(j + 1) * 512], ALU.add),
                     reads=[pr, "abias"], writes=[f"MOD{j // 2}"])
                if MODC is not None and j < 4:
                    pc, pcr = psc.get()
                    for k in range(8):
                        P.op("pe", lambda e, pc=pc, aw=aw, k=k: e.matmul(pc[:], cxT[:, k, :], aw[:, k, :],
                                                                          start=(k == 0), stop=(k == 7)),
                             reads=[ar, "cbx"], writes=[pcr])
                    P.op("dve", lambda e, pc=pc, j=j: e.tensor_tensor(MODC[:, j * 512:(j + 1) * 512], pc[:],
                                                                       abias[:, j * 512:(j + 1) * 512], ALU.add),
                         reads=[pcr, "abias"], writes=[f"MODC{j // 2}"])
            P.op("dve", lambda e: e.scalar_tensor_tensor(modsec("G1"), modsec("G1"), 1.0, gm[:], ALU.add, ALU.mult),
                 reads=["MOD1", "gm"], writes=["MOD1"])
            P.op("dve", lambda e: e.scalar_tensor_tensor(modsec("G2"), modsec("G2"), 1.0, gf[:], ALU.add, ALU.mult),
                 reads=["MOD4", "gf"], writes=["MOD4"])
            if MODC is not None:
                P.op("dve", lambda e: e.scalar_tensor_tensor(MODC[:, D:2 * D], MODC[:, D:2 * D], 1.0, gm[:],
                                                             ALU.add, ALU.mult),
                     reads=["MODC1", "gm"], writes=["MODC1"])
            P.barrier()

    def norm_mod(es_rings, xt, xr, G, S, gres, out, outr, tag, add_eng="pool"):
        junk, jr = es_rings["junk"].get()
        st, sr = es_rings["stat"].get()
        t1, t1r = es_rings["t1"].get()
        P.op("act", lambda e: e.activation(junk[:], xt, AF.Square, accum_out=st[:, 0:1]),
             reads=[xr], writes=[jr, sr])
        P.op("act", lambda e: e.activation(st[:, 1:2], st[:, 0:1], AF.Ln, bias=cols[:, 0:1], scale=1.0 / D),
             reads=[sr, "cols"], writes=[sr])
        P.op("act", lambda e: e.activation(st[:, 2:3], st[:, 1:2], AF.Exp, scale=-0.5), reads=[sr], writes=[sr])
        P.op("dve", lambda e: e.scalar_tensor_tensor(t1[:], xt, st[:, 2:3], G, ALU.mult, ALU.mult),
             reads=[xr, sr] + gres, writes=[t1r])
        P.op(add_eng, lambda e: e.tensor_tensor(out, t1[:], S, ALU.add), reads=[t1r] + gres, writes=[outr])

    def transpose_tile(es_rings, src_tile, srcr, dst_ap, dstr, ident, dt):
        pt, ptr = es_rings["pT"].get()
        for k in range(8):
            P.op("pe", lambda e, k=k: e.transpose(pt[:, k * 128:(k + 1) * 128], src_tile[:, k * 128:(k + 1) * 128],
                                                    ident[:]),
                 reads=[srcr, "identbf", "ident32"], writes=[ptr])
        P.op("act", lambda e: e.copy(dst_ap, pt[:].rearrange("p (k t) -> p k t", k=8)), reads=[ptr], writes=[dstr])

    def layer0():
        with ExitStack() as esL:
          with ExitStack() as esAB:
            MODC = sb(esAB, "MODC", [128, 2 * D], F32)
            with ExitStack() as esA:
                cxT = sb(esA, "cxT", [128, 8, 128], F32)
                make_cb(esA, cctx_d, cxT, "x")
                phase_mods(0, MODC, cxT)
            with ExitStack() as es:
                win = sb(es, "win", [128, 8, 2560], BF16)
                qkg = sb(es, "qkg", [128, 2], F32)
                for k in range(8):
                    P.dma("pool", win[:, k, :], win_d[k * 128:(k + 1) * 128, :], writes=["win"])
                P.dma("sp", qkg[:], qkg_d, writes=["qkg"])
                rings = dict(
                    junk=Ring(es, nc, "junk", [128, D], F32, 1),
                    stat=Ring(es, nc, "stat", [128, 4], F32, 4),
                    t1=Ring(es, nc, "t1", [128, D], F32, 2),
                    pT=Ring(es, nc, "pT", [128, D], BF16, 2, psum=True),
                )
                xr_ = Ring(es, nc, "xt", [128, D], F32, 4)
                hbr = Ring(es, nc, "hbf", [128, D], BF16, 5)
                hTr = Ring(es, nc, "hT", [128, 8, 512], BF16, 3)
                ppr = Ring(es, nc, "pp", [128, 512], F32, 4, psum=True)
                ssr = Ring(es, nc, "ssp", [128, 512], F32, 2, psum=True)
                sqr = Ring(es, nc, "sq", [128, 512], BF16, 2)
                rsr = Ring(es, nc, "rs", [128, 512], F32, 2)
                rdr = Ring(es, nc, "rd", [128, 512], F32, 2)
                qor = Ring(es, nc, "qo", [128, 512], BF16, 3)
                stg = Ring(es, nc, "stg", [128, 512], F32, 3)
                vor = Ring(es, nc, "vo", [128, 512], BF16, 2)

                def proj_prep(src_d, tok0, ntok, G, S, gres):
                    nt = ntok // 128
                    hT, hTres = hTr.get()
                    hbs = []
                    for t in range(nt):
                        xt, xres = xr_.get()
                        P.dma("act", xt[:], src_d[tok0 + t * 128: tok0 + (t + 1) * 128, :], writes=[xres])
                        hb, hbres = hbr.get()
                        norm_mod(rings, xt[:], xres, G, S, gres, hb[:], hbres, "b", add_eng="dve")
                        hbs.append((hb, hbres))
                    for t in range(nt):
                        hb, hbres = hbs[t]
                        transpose_tile(rings, hb, hbres, hT[:, :, t * 128:(t + 1) * 128], hTres, identbf, BF16)
                    return hT, hTres

                def proj_compute(hT, hTres, ntok, is_ctx, col0):
                    nt = ntok // 128
                    fm = []
                    if not is_ctx:
                        fm += [("q", c) for c in range(4)]
                    fm += [("k", c) for c in range(4)]
                    fm += [("xb", c) for c in range(4)]
                    if not is_ctx:
                        fm += [("gb", c) for c in range(4)]
                    base = {"q": 0, "k": 512, "xb": 1536, "gb": 2048}
                    pending = None
                    for (kind, c) in fm:
                        wc = base[kind] + c * 128
                        pp, ppres = ppr.get()
                        for k in range(8):
                            P.op("pe", lambda e, pp=pp, k=k, wc=wc: e.matmul(pp[:, 0:ntok], win[:, k, wc:wc + 128],
                                                                              hT[:, k, 0:ntok], start=(k == 0),
                                                                              stop=(k == 7)),
                                 reads=["win", hTres], writes=[ppres])
                        def post(kind=kind, c=c, pp=pp, ppres=ppres):
                            if kind in ("q", "k"):
                                sq, sqres = sqr.get()
                                P.op("act", lambda e, pp=pp, sq=sq: e.activation(sq[:, 0:ntok], pp[:, 0:ntok], AF.Square),
                                     reads=[ppres], writes=[sqres])
                                ssp, sspres = ssr.get()
                                P.op("pe", lambda e, ssp=ssp, sq=sq: e.matmul(ssp[:, 0:ntok], blkbf[:], sq[:, 0:ntok],
                                                                              start=True, stop=True),
                                     reads=[sqres, "blkbf"], writes=[sspres])
                                rs, rsres = rsr.get()
                                if kind == "q":
                                    P.op("act", lambda e, rs=rs, ssp=ssp: e.activation(rs[:, 0:ntok], ssp[:, 0:ntok], AF.Ln,
                                                                                       bias=cols[:, 1:2], scale=1.0),
                                         reads=[sspres, "cols"], writes=[rsres])
                                else:
                                    P.op("act", lambda e, rs=rs, ssp=ssp: e.activation(rs[:, 0:ntok], ssp[:, 0:ntok], AF.Ln,
                                                                                       bias=cols[:, 0:1], scale=1.0 / 64),
                                         reads=[sspres, "cols"], writes=[rsres])
                                rd, rdres = rdr.get()
                                P.op("act", lambda e, rd=rd, rs=rs: e.activation(rd[:, 0:ntok], rs[:, 0:ntok], AF.Exp, scale=-0.5),
                                     reads=[rsres], writes=[rdres])
                                qo, qores = qor.get()
                                gi = 0 if kind == "q" else 1
                                P.op("dve", lambda e, qo=qo, pp=pp, rd=rd, gi=gi: e.scalar_tensor_tensor(
                                    qo[:, 0:ntok], pp[:, 0:ntok], qkg[:, gi:gi + 1], rd[:, 0:ntok], ALU.mult, ALU.mult),
                                     reads=[ppres, rdres, "qkg"], writes=[qores])
                                dst = qT_d if kind == "q" else kT_d
                                P.dma("sp", dst[c * 128:(c + 1) * 128, col0:col0 + ntok], qo[:, 0:ntok],
                                      reads=[qores], writes=[f"{kind}T_d_{c}_{col0}"])
                            else:
                                sg, sgres = stg.get()
                                P.op("act", lambda e, sg=sg, pp=pp: e.copy(sg[:, 0:ntok], pp[:, 0:ntok]),
                                     reads=[ppres], writes=[sgres])
                                dst = xbT_d if kind == "xb" else gbT_d
                                P.dma("sp", dst[c * 128:(c + 1) * 128, col0:col0 + ntok], sg[:, 0:ntok],
                                      reads=[sgres], writes=[f"{kind}T_d_{c}_{col0}"])
                        if pending is not None:
                            pending()
                        pending = post
                    if pending is not None:
                        pending()
                    for t in range(nt):
                        pp, ppres = ppr.get()
                        for k in range(8):
                            P.op("pe", lambda e, pp=pp, k=k, t=t: e.matmul(pp[:], hT[:, k, t * 128:(t + 1) * 128],
                                                                            win[:, k, 1024:1536], start=(k == 0),
                                                                            stop=(k == 7)),
                                 reads=["win", hTres], writes=[ppres])
                        vo, vores = vor.get()
                        P.op("dve", lambda e, vo=vo, pp=pp: e.tensor_copy(vo[:], pp[:]), reads=[ppres], writes=[vores])
                        P.dma("sp", v_d[col0 + t * 128: col0 + (t + 1) * 128, :], vo[:], reads=[vores], writes=[f"v_d_{col0}_{t}"])

                preps = {}
                preps[-1] = proj_prep(ctx_d, 0, CT, MODC[:, D:2 * D], MODC[:, 0:D], ["MODC0", "MODC1"])
                preps[0] = proj_prep(x_d, 0, 512, modsec("G1"), modsec("S1"), ["MOD0", "MOD1"])
                for tc in range(-1, 8):
                    if tc + 2 < 8:
                        preps[tc + 2] = proj_prep(x_d, (tc + 2) * 512, 512, modsec("G1"), modsec("S1"), ["MOD0", "MOD1"])
                    cur = preps.pop(tc)
                    if tc < 0:
                        proj_compute(cur[0], cur[1], CT, True, N)
                    else:
                        proj_compute(cur[0], cur[1], 512, False, tc * 512)
                P.barrier()
          with ExitStack() as es:
              qTp = Ring(es, nc, "qTp", [128, N], BF16, 2)
              kTp = Ring(es, nc, "kTp", [128, N + CT], BF16, 2)
              Vp = Ring(es, nc, "Vp", [128, NT + 2, 128], BF16, 2)
              BTp = Ring(es, nc, "BTp", [128, 2, 14, 64], F32, 2)
              SPr = Ring(es, nc, "SP", [128, 2, 8, 64], F32, 3, psum=True)
              OTr = Ring(es, nc, "OT", [128, 256], F32, 2, psum=True)
              Sbr = Ring(es, nc, "Sb", [128, 2, 8, 64], F32, 3)
              PTr = Ring(es, nc, "PT", [128, 2, 8, 64], BF16, 3)
              rcr = Ring(es, nc, "rc", [128, 128], F32, 2)
              asr = Ring(es, nc, "ast", [128, 512], BF16, 3)
              units = []
              for p in range(4):
                  for r in range(64):
                      units.append(dict(p=p, r=r))
              pair_bufs = {}

              def load_pair(p):
                  qT, qres = qTp.get()
                  kT, kres = kTp.get()
                  V, vres = Vp.get()
                  BT, bres = BTp.get()
                  P.dma("sp", qT[:], qT_d[p * 128:(p + 1) * 128, :], writes=[qres])
                  P.dma("sp", kT[:], kT_d[p * 128:(p + 1) * 128, :], writes=[kres])
                  for t4 in range(0, NT + 2, 2):
                      P.dma("sp", V[:, t4:t4 + 2, :],
                            v_d[t4 * 128:(t4 + 2) * 128, p * 128:(p + 1) * 128].rearrange("(t q) c -> q t c", q=128),
                            writes=[vres])
                  P.dma("sp", BT[:], bt_d[p], writes=[bres])
                  pair_bufs[p] = (qT, qres, kT, kres, V, vres, BT, bres)

              def stageA(u):
                  p, r = u["p"], u["r"]
                  if p not in pair_bufs:
                      load_pair(p)
                  qT, qres, kT, kres, V, vres, BT, bres = pair_bufs[p]
                  rs_ = min(max(r - 4, 0), 56)
                  if rs_ % 2 == 0:
                      tile0, nslot, parts, d0 = rs_ // 2, 4, [(0, 128)] * 4, rs_ - r + 7
                  else:
                      tile0, nslot, d0 = (rs_ - 1) // 2, 5, rs_ - 1 - r + 7
                      parts = [(64, 128), (0, 128), (0, 128), (0, 128), (0, 64)]
                  SP, spres = SPr.get()
                  u.update(tile0=tile0, nslot=nslot, parts=parts, d0=d0, SP=SP, spres=spres)
                  qs = slice(r * 64, (r + 1) * 64)
                  for h in range(2):
                      hp = slice(h * 64, (h + 1) * 64)
                      for s in range(nslot + 2):
                          tk = (tile0 + s) * 128 if s < nslot else N + (s - nslot) * 128
                          P.op("pe", lambda e, SP=SP, h=h, s=s, hp=hp, tk=tk, kT=kT, qT=qT, qs=qs: e.matmul(
                              SP[:, h, s, :], kT[hp, tk:tk + 128], qT[hp, qs], start=True, stop=True),
                               reads=[kres, qres], writes=[spres])

              def stageBC(u):
                  qT, qres, kT, kres, V, vres, BT, bres = pair_bufs[u["p"]]
                  SP, spres, nslot, d0 = u["SP"], u["spres"], u["nslot"], u["d0"]
                  Sb, sbres = Sbr.get()
                  for h in range(2):
                      P.op("dve", lambda e, Sb=Sb, SP=SP, h=h, BT=BT, d0=d0, nslot=nslot: e.tensor_tensor(
                          Sb[:, h, 0:nslot, :], SP[:, h, 0:nslot, :], BT[:, h, d0:d0 + 2 * nslot - 1:2, :], ALU.add),
                           reads=[spres, bres], writes=[sbres])
                  PT, ptres = PTr.get()
                  P.op("act", lambda e, PT=PT, Sb=Sb, nslot=nslot: e.activation(PT[:, :, 0:nslot, :], Sb[:, :, 0:nslot, :], AF.Exp),
                       reads=[sbres], writes=[ptres])
                  for h in range(2):
                      P.op("act", lambda e, PT=PT, SP=SP, h=h, nslot=nslot: e.activation(
                          PT[:, h, nslot:nslot + 2, :], SP[:, h, nslot:nslot + 2, :], AF.Exp),
                           reads=[spres], writes=[ptres])
                  u.update(PT=PT, ptres=ptres)

              ast_state = {}

              def stageDE(u):
                  p, r = u["p"], u["r"]
                  qT, qres, kT, kres, V, vres, BT, bres = pair_bufs[p]
                  PT, ptres, nslot, parts, tile0 = u["PT"], u["ptres"], u["nslot"], u["parts"], u["tile0"]
                  OT, otres = OTr.get()
                  ns = nslot + 2
                  for which in range(2):
                      for s in range(ns):
                          if s < nslot:
                              lo, hi = parts[s]
                              vt = tile0 + s
                          else:
                              lo, hi = 0, 128
                              vt = NT + (s - nslot)
                          if which == 0:
                              P.op("pe", lambda e, OT=OT, PT=PT, s=s, lo=lo, hi=hi, vt=vt, V=V, ns=ns: e.matmul(
                                  OT[:, 0:128].rearrange("p (h q) -> p h q", h=2), V[lo:hi, vt, :],
                                  PT[lo:hi, :, s, :], start=(s == 0), stop=(s == ns - 1)),
                                   reads=[vres, ptres], writes=[otres])
                          else:
                              P.op("pe", lambda e, OT=OT, PT=PT, s=s, lo=lo, hi=hi, ns=ns: e.matmul(
                                  OT[:, 128:256].rearrange("p (h q) -> p h q", h=2), onesbf[lo:hi, :],
                                  PT[lo:hi, :, s, :], start=(s == 0), stop=(s == ns - 1)),
                                   reads=["onesbf", ptres], writes=[otres])
                  rc, rcres = rcr.get()
                  P.op("dve", lambda e, rc=rc, OT=OT: e.reciprocal(rc[:], OT[:, 128:256]), reads=[otres], writes=[rcres])
                  if r % 8 == 0:
                      ast_state["cur"] = asr.get()
                  ast, astres = ast_state["cur"]
                  rr = r % 8
                  for h in range(2):
                      hp = slice(h * 64, (h + 1) * 64)
                      P.op("dve", lambda e, ast=ast, OT=OT, rc=rc, hp=hp, rr=rr: e.tensor_tensor(
                          ast[hp, rr * 64:(rr + 1) * 64], OT[hp, hp], rc[hp, hp], ALU.mult),
                           reads=[otres, rcres], writes=[astres])
                  if rr == 7:
                      r0 = r - 7
                      P.dma("act", cat_d[0][p * 128:(p + 1) * 128, r0 * 64:(r0 + 8) * 64], ast[:],
                            reads=[astres], writes=[f"cat0a_{p}_{r}"])

              nu = len(units)
              for step in range(nu + 2):
                  if step < nu:
                      stageA(units[step])
                  if 0 <= step - 1 < nu:
                      stageBC(units[step - 1])
                  if 0 <= step - 2 < nu:
                      stageDE(units[step - 2])
              P.barrier()
          with ExitStack() as es:
              L = N + CT
              cw = sb(es, "cw", [128, 4, 4], F32)
              cbv = sb(es, "cbv", [128, 4], F32)
              wbd = sb(es, "wbd", [128, 16, 128], BF16)
              gbias = sb(es, "gbias", [128, 16], F32)
              lam = sb(es, "lam", [128, 8], F32)
              m8 = sb(es, "m8", [128, 8], F32)
              m16 = sb(es, "m16", [128, 8], F32)
              P.dma("sp", cw[:], cw_d, writes=["cw"])
              P.dma("sp", cbv[:], cb_d, writes=["cbv"])
              P.dma("pool", wbd[:], wbd_d, writes=["wbd"])
              P.dma("sp", gbias[:], gbias_d, writes=["gbias"])
              P.dma("sp", lam[:], lam_d, writes=["lam"])
              P.op("act", lambda e: e.activation(m8[:], lam[:], AF.Exp, scale=-1.0), reads=["lam"], writes=["m8"])
              P.op("act", lambda e: e.activation(m8[:], m8[:], AF.Ln, bias=cols[:, 2:3], scale=1.0),
                   reads=["m8", "cols"], writes=["m8"])
              P.op("dve", lambda e: e.tensor_scalar(m16[:], m8[:], -16.0, None, ALU.mult), reads=["m8"], writes=["m16"])
              P.op("dve", lambda e: e.tensor_scalar(m8[:], m8[:], -8.0, None, ALU.mult), reads=["m8", "m16"], writes=["m8"])
              xpad = sb(es, "xpad", [128, N + 4], F32)
              xpc = sb(es, "xpc", [128, CT + 4], F32)
              xc = sb(es, "xc", [128, L], F32)
              xcb = sb(es, "xcb", [128, L], BF16)
              rt = sb(es, "rt", [128, L], F32)
              it = sb(es, "it", [128, L], F32)
              at = sb(es, "at", [128, L], F32)
              hf = sb(es, "hf", [128, L], F32)
              hb_ = sb(es, "hb", [128, L], F32)
              gbt = sb(es, "gbt", [128, N], F32)
              lro = sb(es, "lro", [128, N], BF16)
              gpr = Ring(es, nc, "gps", [128, 512], F32, 4, psum=True)
              P.op("pool", lambda e: e.memset(xpad[:, 0:2], 0.0), writes=["xpad_l"])
              P.op("pool", lambda e: e.memset(xpad[:, N + 2:N + 4], 0.0), writes=["xpad_r"])
              P.op("pool", lambda e: e.memset(xpc[:, 0:2], 0.0), writes=["xpc_l"])
              P.op("pool", lambda e: e.memset(xpc[:, CT + 2:CT + 4], 0.0), writes=["xpc_r"])
              pieces = [(0, CT)] + [(CT + i * 512, 512) for i in range(8)]
              for c in range(4):
                  P.dma("sp", xpad[:, 2:N + 2], xbT_d[c * 128:(c + 1) * 128, 0:N], reads=["xbT_d"], writes=["xpad"])
                  P.dma("sp", xpc[:, 2:CT + 2], xbT_d[c * 128:(c + 1) * 128, N:N + CT], reads=["xbT_d"], writes=["xpc"])
                  P.dma("sp", gbt[:], gbT_d[c * 128:(c + 1) * 128, :], reads=["gbT_d"], writes=["gbt"])
                  for (src, srcres, n, o0) in ((xpc, ["xpc", "xpc_l", "xpc_r"], CT, 0),
                                               (xpad, ["xpad", "xpad_l", "xpad_r"], N, CT)):
                      P.op("dve", lambda e, src=src, n=n, o0=o0, c=c: e.tensor_scalar(
                          xc[:, o0:o0 + n], src[:, 0:n], cw[:, c, 0:1], cbv[:, c:c + 1], ALU.mult, ALU.add),
                           reads=srcres + ["cw", "cbv"], writes=["xc"])
                      for tap in range(1, 4):
                          P.op("dve", lambda e, src=src, n=n, o0=o0, c=c, tap=tap: e.scalar_tensor_tensor(
                              xc[:, o0:o0 + n], src[:, tap:tap + n], cw[:, c, tap:tap + 1], xc[:, o0:o0 + n],
                              ALU.mult, ALU.add), reads=srcres + ["cw", "xc"], writes=["xc"])
                  P.op("act", lambda e: e.copy(xcb[:], xc[:]), reads=["xc"], writes=["xcb"])
                  for d in range(2):
                      ir = (0 * 2 + d) * 4 + c
                      ii = (1 * 2 + d) * 4 + c
                      for (o0, n) in pieces:
                          g1, g1r = gpr.get()
                          P.op("pe", lambda e, g1=g1, o0=o0, n=n, ir=ir: e.matmul(g1[:, 0:n], wbd[:, ir, :], xcb[:, o0:o0 + n],
                                                                                  start=True, stop=True),
                               reads=["wbd", "xcb"], writes=[g1r])
                          P.op("act", lambda e, g1=g1, o0=o0, n=n, ir=ir: e.activation(
                              rt[:, o0:o0 + n], g1[:, 0:n], AF.Sigmoid, bias=gbias[:, ir:ir + 1], scale=1.0),
                               reads=[g1r, "gbias"], writes=["rt"])
                          g2, g2r = gpr.get()
                          P.op("pe", lambda e, g2=g2, o0=o0, n=n, ii=ii: e.matmul(g2[:, 0:n], wbd[:, ii, :], xcb[:, o0:o0 + n],
                                                                                  start=True, stop=True),
                               reads=["wbd", "xcb"], writes=[g2r])
                          P.op("act", lambda e, g2=g2, o0=o0, n=n, ii=ii: e.activation(
                              it[:, o0:o0 + n], g2[:, 0:n], AF.Sigmoid, bias=gbias[:, ii:ii + 1], scale=1.0),
                               reads=[g2r, "gbias"], writes=["it"])
                      lc = d * 4 + c
                      P.op("act", lambda e, lc=lc: e.activation(at[:], rt[:], AF.Exp, scale=m8[:, lc:lc + 1]),
                           reads=["rt", "m8"], writes=["at"])
                      P.op("act", lambda e, lc=lc: e.activation(rt[:], rt[:], AF.Exp, scale=m16[:, lc:lc + 1]),
                           reads=["rt", "m16"], writes=["rt"])
                      P.op("act", lambda e: e.activation(rt[:], rt[:], AF.Sqrt, bias=cols[:, 2:3], scale=-1.0),
                           reads=["rt", "cols"], writes=["rt"])
                      P.op("dve", lambda e: e.tensor_tensor(it[:], it[:], xc[:], ALU.mult), reads=["it", "xc"], writes=["it"])
                      P.op("dve", lambda e: e.tensor_tensor(it[:], it[:], rt[:], ALU.mult), reads=["it", "rt"], writes=["it"])
                      if d == 0:
                          prev = None
                          for (o0, n) in pieces:
                              init = 0.0 if prev is None else hf[:, o0 - 1:o0]
                              P.op("dve", lambda e, o0=o0, n=n, init=init: e.tensor_tensor_scan(
                                  hf[:, o0:o0 + n], at[:, o0:o0 + n], it[:, o0:o0 + n], init, ALU.mult, ALU.add),
                                   reads=["at", "it", "hf"], writes=["hf"])
                              prev = o0
                      else:
                          order = [(0, CT)] + [(CT + i * 512, 512) for i in reversed(range(8))]
                          first = True
                          for idx, (o0, n) in enumerate(order):
                              if idx == 0:
                                  init = 0.0
                              elif idx == 1:
                                  init = hb_[:, 0:1]
                              else:
                                  init = hb_[:, o0 + n:o0 + n + 1]
                              P.op("dve", lambda e, o0=o0, n=n, init=init: e.tensor_tensor_scan(
                                  hb_[:, o0:o0 + n][:, ::-1], at[:, o0:o0 + n][:, ::-1], it[:, o0:o0 + n][:, ::-1],
                                  init, ALU.mult, ALU.add), reads=["at", "it", "hb"], writes=["hb"])
                  P.op("dve", lambda e: e.tensor_tensor(hf[:, CT:L], hf[:, CT:L], hb_[:, CT:L], ALU.add),
                       reads=["hf", "hb"], writes=["hf"])
                  P.op("act", lambda e: e.activation(gbt[:], gbt[:], AF.Gelu_apprx_tanh), reads=["gbt"], writes=["gbt"])
                  P.op("dve", lambda e: e.tensor_tensor(lro[:], gbt[:], hf[:, CT:L], ALU.mult),
                       reads=["gbt", "hf"], writes=["lro"])
                  P.dma("sp", cat_d[0][512 + c * 128:512 + (c + 1) * 128, :], lro[:], reads=["lro"], writes=[f"cat0l{c}"])
              P.barrier()
          return phase_E(0, cat_d[0], wout_d, x_d, xmid_d if LAYERS > 1 else out_d)

    def phase_E(l, catd, w_d, xin_d, xout_d):
        final = []
        X = mybir.AxisListType.X
        with ExitStack() as esE:
            W12 = sb(esE, "W12", [128, 2, 32], F32)
            POSI = sb(esE, "POSI", [128, 2, 32], I32)
            WIDX = sb(esE, "WIDX", [128, 32], I32)
            WIDXD = sb(esE, "WIDXD", [128, 4, 32], I32)
            zt = sb(esE, "zt", [128, D], BF16)
            P.op("pool", lambda e: e.memset(zt[:], 0.0), writes=["zt"])
            for c8 in range(8):
                P.dma("pool", xs_d[c8 * 2048:(c8 + 1) * 2048, :].rearrange("(p j) d -> p j d", p=128),
                      zt[:].unsqueeze(1).to_broadcast([128, 16, D]), reads=["zt"], writes=[f"xs_zero{c8}"])
            with ExitStack() as es:
                H2B = sb(es, "H2B", [128, 32, D], BF16)
                SC = sb(es, "SC", [128, 32 * NE], F32)
                wo = sb(es, "wo", [128, 8, D], BF16)
                for k in range(8):
                    P.dma("pool", wo[:, k, :], w_d[k * 128:(k + 1) * 128, :], writes=["wo"])
                esr = ExitStack()
                rings = dict(
                    junk=Ring(esr, nc, "junkE", [128, D], BF16, 1),
                    stat=Ring(esr, nc, "statE", [128, 4], F32, 4),
                    t1=Ring(esr, nc, "t1E", [128, D], F32, 2),
                )
                catr = Ring(esr, nc, "catT", [128, 8, 512], BF16, 2)
                xr_ = Ring(esr, nc, "xtE", [128, D], F32, 2)
                tmr = Ring(esr, nc, "tmE", [128, D], F32, 2)
                x1r = Ring(esr, nc, "x1E", [128, D], F32, 2)
                h2r = Ring(esr, nc, "h2E", [128, D], F32, 3)
                h32r = Ring(esr, nc, "h32E", [128, 8, 128], F32, 2)
                mpr = Ring(esr, nc, "mixp", [128, 512], F32, 2, psum=True)
                tpr = Ring(esr, nc, "tpE", [128, 8, 128], F32, 1, psum=True)
                lgr = Ring(esr, nc, "lgE", [128, NE], F32, 2, psum=True)
                catv = catd.rearrange("(k p) t -> p k t", p=128)
                cat_state = {}
                tiles = {}
                tiles2 = {}

                def part1(t):
                    tcn, tt = t // 4, t % 4
                    if tt == 0:
                        catT, cres = catr.get()
                        P.dma("sp", catT[:], catv[:, :, tcn * 512:(tcn + 1) * 512], writes=[cres])
                        cat_state["cur"] = (catT, cres)
                    catT, cres = cat_state["cur"]
                    tok = t * 128
                    xt, xres = xr_.get()
                    P.dma("sp", xt[:], xin_d[tok:tok + 128, :], writes=[xres])
                    tm, tmres = tmr.get()
                    for half in range(2):
                        mp, mpres = mpr.get()
                        for k in range(8):
                            P.op("pe", lambda e, mp=mp, k=k, tt=tt, half=half, catT=catT: e.matmul(
                                mp[:], catT[:, k, tt * 128:(tt + 1) * 128], wo[:, k, half * 512:(half + 1) * 512],
                                start=(k == 0), stop=(k == 7)), reads=[cres, "wo"], writes=[mpres])
                        P.op("dve", lambda e, mp=mp, tm=tm, half=half: e.tensor_tensor(
                            tm[:, half * 512:(half + 1) * 512], mp[:], MOD[:, 2 * D + half * 512:2 * D + (half + 1) * 512],
                            ALU.mult), reads=[mpres, "MOD2"], writes=[tmres])
                    x1, x1res = x1r.get()
                    P.op("dve", lambda e, tm=tm, xt=xt, x1=x1: e.tensor_tensor(x1[:], tm[:], xt[:], ALU.add),
                         reads=[tmres, xres], writes=[x1res])
                    P.dma("act", x1_d[tok:tok + 128, :], x1[:], reads=[x1res], writes=[f"x1d_{t}"])
                    h2, h2res = h2r.get()
                    norm_mod(rings, x1[:], x1res, modsec("G2"), modsec("S2"), ["MOD3", "MOD4"], h2[:], h2res, "e", add_eng="dve")
                    P.op("act", lambda e, h2=h2, t=t: e.copy(H2B[:, t, :], h2[:]), reads=[h2res], writes=[f"H2B{t}"])
                    tiles[t] = (h2, h2res)

                def part2(t):
                    h2, h2res = tiles.pop(t)
                    tp, tpres = tpr.get()
                    for k in range(8):
                        P.op("pe", lambda e, tp=tp, h2=h2, k=k: e.transpose(tp[:, k, :], h2[:, k * 128:(k + 1) * 128],
                                                                            ident32[:]),
                             reads=[h2res, "ident32"], writes=[tpres])
                    h32, h32res = h32r.get()
                    P.op("act", lambda e, h32=h32, tp=tp: e.copy(h32[:], tp[:]), reads=[tpres], writes=[h32res])
                    tiles2[t] = (h32, h32res)

                def part2b(t):
                    h32, h32res = tiles2.pop(t)
                    lg, lgres = lgr.get()
                    for k in range(8):
                        P.op("pe", lambda e, lg=lg, h32=h32, k=k: e.matmul(lg[:], h32[:, k, :], rw32[:, k, :],
                                                                            start=(k == 0), stop=(k == 7)),
                             reads=[h32res, "rw32"], writes=[lgres])
                    P.op("act", lambda e, lg=lg, t=t: e.activation(SC[:, t * NE:(t + 1) * NE], lg[:], AF.Sigmoid),
                         reads=[lgres], writes=["route"])

                part1(0)
                part1(1)
                for t in range(32):
                    part2(t)
                    if t + 2 < 32:
                        part1(t + 2)
                    part2b(t)
                P.barrier()
                esr.close()
                RT = ["route"]

                def T512(name, dt=F32):
                    return sb(es, name, [128, 512], dt)
                SEL = T512("SEL"); W2 = T512("W2"); SELM = T512("SELM"); OH1 = T512("OH1"); OH2 = T512("OH2")
                POS = T512("POS"); PA = T512("PA"); PB = T512("PB"); TSB = T512("TSB"); Mb = T512("Mb", BF16)
                GM = sb(es, "GM", [128, 128], F32); GM2 = sb(es, "GM2", [128, 128], F32); GS = sb(es, "GS", [128, 128], F32)
                S32 = sb(es, "S32", [128, 8, 32], F32)
                S16 = sb(es, "S16", [128, 6, 16], F32)
                THR = sb(es, "THR", [128, 8], F32)
                CMPC = sb(es, "CMPC", [128, 16, 8], F32)
                PID = sb(es, "PID", [128, 8], F32)
                POSF = sb(es, "POSF", [128, 2, 32], F32)
                WF = sb(es, "WF", [128, 5, 32], F32)
                Lst = sb(es, "Lst", [128, 128], F32)
                Lsb = sb(es, "Lsb", [128, 128], BF16)
                Rps = psm(es, "Rps", [128, 512], F32)
                Tps = psm(es, "Tps", [128, 512], F32)

                def dv(fn, reads=RT, writes=RT):
                    P.op("dve", fn, reads=reads, writes=writes)
                v3 = lambda a: a[:].rearrange("p (a k) -> p a k", k=4)
                v16 = lambda a: a[:].rearrange("p (t e) -> p t e", e=16)
                g4 = lambda a: a[:].rearrange("p (t g) -> p t g", g=4)
                P.op("pool", lambda e: e.iota(S32[:, 7, :], [[1, 32]], base=0, channel_multiplier=0,
                                              allow_small_or_imprecise_dtypes=True), reads=RT, writes=RT)
                P.op("pool", lambda e: e.iota(THR[:], [[512, 8]], base=0, channel_multiplier=0,
                                              allow_small_or_imprecise_dtypes=True), reads=RT, writes=RT)
                for f in range(5):
                    P.op("pool", lambda e, f=f: e.iota(PID[:, f:f + 1], [[0, 1]], base=(128 * f + l * 8192) if f < 4 else l * 2048, channel_multiplier=1,
                                                       allow_small_or_imprecise_dtypes=True), reads=RT, writes=RT)
                P.op("pool", lambda e: e.memset(S16[:, 4, :], 1.0), reads=RT, writes=RT)
                P.op("pool", lambda e: e.memset(Lst[:], 1.0), reads=RT, writes=RT)
                P.op("pool", lambda e: e.affine_select(Lst[:], Lst[:], [[1, 128]], ALU.is_gt, 0.0, base=0,
                                                       channel_multiplier=-1), reads=RT, writes=RT)
                dv(lambda e: e.tensor_copy(Lsb[:], Lst[:]))
                dv(lambda e: e.tensor_tensor(v16(SEL), v16(SC), rbias[:].unsqueeze(1).to_broadcast([128, 32, NE]), ALU.add),
                   reads=RT + ["rbias"])
                dv(lambda e: e.tensor_reduce(GM[:], v3(SEL), X, ALU.max))
                dv(lambda e: e.tensor_tensor(v3(W2), v3(SEL), GM[:].unsqueeze(2).to_broadcast([128, 128, 4]), ALU.is_equal))
                dv(lambda e: e.scalar_tensor_tensor(W2[:], W2[:], -1000.0, SEL[:], ALU.mult, ALU.add))
                dv(lambda e: e.tensor_reduce(GM2[:], v3(W2), X, ALU.max))
                dv(lambda e: e.tensor_tensor(GS[:], GM[:], GM2[:], ALU.add))
                dv(lambda e: e.tensor_reduce(S32[:, 0, :], g4(GS), X, ALU.max))
                dv(lambda e: e.tensor_tensor(g4(GS), g4(GS), S32[:, 0, :].unsqueeze(2).to_broadcast([128, 32, 4]), ALU.is_equal))
                dv(lambda e: e.tensor_scalar(GS[:], GS[:], -1.0, 1000.0, ALU.add, ALU.mult))
                dv(lambda e: e.tensor_tensor(v3(SELM), v3(SEL), GS[:].unsqueeze(2).to_broadcast([128, 128, 4]), ALU.add))
                dv(lambda e: e.tensor_reduce(S32[:, 1, :], v16(SELM), X, ALU.max))
                dv(lambda e: e.tensor_tensor(v16(OH1), v16(SELM), S32[:, 1, :].unsqueeze(2).to_broadcast([128, 32, NE]), ALU.is_equal))
                dv(lambda e: e.scalar_tensor_tensor(W2[:], OH1[:], -1000.0, SELM[:], ALU.mult, ALU.add))
                dv(lambda e: e.tensor_reduce(S32[:, 2, :], v16(W2), X, ALU.max))
                dv(lambda e: e.tensor_tensor(v16(OH2), v16(W2), S32[:, 2, :].unsqueeze(2).to_broadcast([128, 32, NE]), ALU.is_equal))
                dv(lambda e: e.tensor_tensor(SEL[:], OH1[:], SC[:], ALU.mult))
                dv(lambda e: e.tensor_reduce(S32[:, 3, :], v16(SEL), X, ALU.add))
                dv(lambda e: e.tensor_tensor(SEL[:], OH2[:], SC[:], ALU.mult))
                dv(lambda e: e.tensor_reduce(S32[:, 4, :], v16(SEL), X, ALU.add))
                dv(lambda e: e.tensor_tensor(S32[:, 5, :], S32[:, 3, :], S32[:, 4, :], ALU.add))
                dv(lambda e: e.reciprocal(S32[:, 5, :], S32[:, 5, :]))
                dv(lambda e: e.tensor_tensor(W12[:, 0, :], S32[:, 3, :], S32[:, 5, :], ALU.mult), writes=RT + ["W12"])
                dv(lambda e: e.tensor_tensor(W12[:, 1, :], S32[:, 4, :], S32[:, 5, :], ALU.mult), writes=RT + ["W12"])
                dv(lambda e: e.tensor_tensor(Mb[:], OH1[:], OH2[:], ALU.add))
                P.op("pe", lambda e: e.matmul(Rps[:], Lsb[:], Mb[:], start=True, stop=True), reads=RT, writes=["Rps"])
                P.op("pe", lambda e: e.matmul(Tps[:], onesbf[:], Mb[:], start=True, stop=True), reads=RT + ["onesbf"], writes=["Tps"])
                dv(lambda e: e.tensor_copy(TSB[:], Tps[:]), reads=RT + ["Tps"])
                dv(lambda e: e.tensor_copy(PA[:], Tps[:]), reads=RT + ["Tps"])
                src, dst = PA, PB
                for s in (1, 2, 4, 8, 16):
                    w = s * NE
                    dv(lambda e, src=src, dst=dst, w=w: e.tensor_tensor(dst[:, w:512], src[:, w:512], src[:, 0:512 - w], ALU.add))
                    dv(lambda e, src=src, dst=dst, w=w: e.tensor_copy(dst[:, 0:w], src[:, 0:w]))
                    src, dst = dst, src
                PINC = src
                dv(lambda e: e.tensor_copy(S16[:, 0, :], PINC[:, 31 * NE:32 * NE]))
                dv(lambda e: e.tensor_tensor(CMPC[:], S16[:, 0, :].unsqueeze(2).to_broadcast([128, 16, 8]),
                                             THR[:].unsqueeze(1).to_broadcast([128, 16, 8]), ALU.is_gt))
                dv(lambda e: e.tensor_reduce(S16[:, 1, :], CMPC[:], X, ALU.add))
                dv(lambda e: e.tensor_tensor_scan(S16[:, 2, :], S16[:, 4, :], S16[:, 1, :], 0.0, ALU.mult, ALU.add))
                dv(lambda e: e.tensor_tensor(S16[:, 3, :], S16[:, 2, :], S16[:, 1, :], ALU.subtract))
                dv(lambda e: e.tensor_scalar(S16[:, 3, :], S16[:, 3, :], 512.0, None, ALU.mult))
                dv(lambda e: e.tensor_tensor(PINC[:], PINC[:], TSB[:], ALU.subtract))
                dv(lambda e: e.tensor_tensor(POS[:], Rps[:], PINC[:], ALU.add), reads=RT + ["Rps"])
                dv(lambda e: e.tensor_tensor(v16(POS), v16(POS), S16[:, 3, :].unsqueeze(1).to_broadcast([128, 32, NE]), ALU.add))
                dv(lambda e: e.tensor_tensor(SEL[:], OH1[:], POS[:], ALU.mult))
                dv(lambda e: e.tensor_reduce(POSF[:, 0, :], v16(SEL), X, ALU.add))
                dv(lambda e: e.tensor_tensor(SEL[:], OH2[:], POS[:], ALU.mult))
                dv(lambda e: e.tensor_reduce(POSF[:, 1, :], v16(SEL), X, ALU.add))
                dv(lambda e: e.tensor_copy(POSI[:], POSF[:]), writes=RT + ["POSI"])
                dv(lambda e: e.tensor_tensor(v16(SEL), S16[:, 2, :].unsqueeze(1).to_broadcast([128, 32, NE]),
                                             S32[:, 7, :].unsqueeze(2).to_broadcast([128, 32, NE]), ALU.is_le))
                dv(lambda e: e.tensor_reduce(S32[:, 6, :], v16(SEL), X, ALU.add))
                dv(lambda e: e.tensor_scalar(S32[:, 5, :], S32[:, 6, :], 16.0, 1.0e6, ALU.is_ge, ALU.mult))
                dv(lambda e: e.tensor_scalar(S32[:, 6, :], S32[:, 6, :], 15.0, None, ALU.min))
                dv(lambda e: e.tensor_scalar(WF[:, 4, :], S32[:, 6, :], 128.0, PID[:, 4:5], ALU.mult, ALU.add))
                dv(lambda e: e.tensor_tensor(WF[:, 4, :], WF[:, 4, :], S32[:, 5, :], ALU.add))
                dv(lambda e: e.tensor_copy(WIDX[:], WF[:, 4, :]), writes=RT + ["WIDX"])
                for f in range(4):
                    dv(lambda e, f=f: e.tensor_scalar(WF[:, f, :], S32[:, 6, :], 512.0, PID[:, f:f + 1], ALU.mult, ALU.add))
                for f in range(4):
                    dv(lambda e, f=f: e.tensor_tensor(WF[:, f, :], WF[:, f, :], S32[:, 5, :], ALU.add))
                dv(lambda e: e.tensor_copy(WIDXD[:], WF[:, 0:4, :]), writes=RT + ["WIDX"])
                for t in range(32):
                    for k in range(2):
                        P.dma("pool", xs_d[:, :], H2B[:, t, :], ind=(POSI[:, k, t:t + 1], None),
                              reads=["POSI", f"H2B{t}"] + [f"xs_zero{c8}" for c8 in range(8)], writes=[f"xs_{t}_{k}"],
                              bounds_check=16383, oob_is_err=False)
                P.barrier()
            with ExitStack() as es:
                xsr = Ring(es, nc, "xsb", [128, 4, D], BF16, 2)
                xTr = Ring(es, nc, "xTm", [128, 8, 512], BF16, 2)
                wgr = Ring(es, nc, "wg", [128, 8 * DFF], BF16, 2)
                wur = Ring(es, nc, "wu", [128, 8 * DFF], BF16, 2)
                wdr = Ring(es, nc, "wd", [128, 4, D], BF16, 2)
                pTr = Ring(es, nc, "pTm", [128, D], BF16, 2, psum=True)
                pgr = Ring(es, nc, "pg", [128, 512], F32, 2, psum=True)
                pur = Ring(es, nc, "pu", [128, 512], F32, 2, psum=True)
                pyr = Ring(es, nc, "py", [128, 512], F32, 2, psum=True)
                sgr = Ring(es, nc, "sg", [128, 512], F32, 3)
                acr = Ring(es, nc, "acT", [128, 4, 512], BF16, 2)
                ysr = Ring(es, nc, "ys", [128, D], F32, 3)
                loaded = {}

                def issue_loads(i):
                    xsb, xsres = xsr.get()
                    P.dma("sp", xsb[:], xs_d[i * 512:(i + 1) * 512, :].rearrange("(j p) d -> p j d", p=128), writes=[xsres])
                    wg, wgres = wgr.get()
                    wu, wures = wur.get()
                    wd, wdres = wdr.get()
                    P.dma("pool", wg[:, :], wg_d[:, :], ind=(None, WIDX[:, i:i + 1]),
                          reads=["WIDX"], writes=[wgres], bounds_check=4095, oob_is_err=False)
                    P.dma("pool", wu[:, :], wu_d[:, :], ind=(None, WIDX[:, i:i + 1]),
                          reads=["WIDX"], writes=[wures], bounds_check=4095, oob_is_err=False)
                    for f in range(4):
                        P.dma("pool", wd[:, f, :], wd_d[:, :], ind=(None, WIDXD[:, f, i:i + 1]), reads=["WIDX"], writes=[wdres],
                              bounds_check=16383, oob_is_err=False)
                    loaded[i] = (xsb, xsres, wg, wgres, wu, wures, wd, wdres)

                xts = {}

                def do_transposes(i):
                    xsb, xsres = loaded[i][0], loaded[i][1]
                    xT, xTres = xTr.get()
                    for j in range(4):
                        pt, ptres = pTr.get()
                        for k in range(8):
                            P.op("pe", lambda e, pt=pt, xsb=xsb, j=j, k=k: e.transpose(
                                pt[:, k * 128:(k + 1) * 128], xsb[:, j, k:D:8], identbf[:]),
                                 reads=[xsres, "identbf"], writes=[ptres])
                        if j % 2 == 0:
                            P.op("act", lambda e, pt=pt, xT=xT, j=j: e.copy(xT[:, :, j * 128:(j + 1) * 128],
                                                                              pt[:].rearrange("p (k t) -> p k t", k=8)),
                                 reads=[ptres], writes=[xTres])
                        else:
                            P.op("dve", lambda e, pt=pt, xT=xT, j=j: e.tensor_copy(xT[:, :, j * 128:(j + 1) * 128],
                                                                                    pt[:].rearrange("p (k t) -> p k t", k=8)),
                                 reads=[ptres], writes=[xTres])
                    xts[i] = (xT, xTres)

                issue_loads(0)
                issue_loads(1)
                do_transposes(0)
                for i in range(32):
                    xsb, xsres, wg, wgres, wu, wures, wd, wdres = loaded[i]
                    xT, xTres = xts.pop(i)
                    for f in range(4):
                        P.op("dve", lambda e, wd=wd, f=f: e.tensor_tensor(wd[:, f, :], wd[:, f, :], modsec("GATE2"), ALU.mult),
                             reads=[wdres, "MOD5"], writes=[wdres])
                    acT, acres = acr.get()
                    for f in range(4):
                        pg, pgres = pgr.get()
                        pu, pures = pur.get()
                        for k in range(8):
                            P.op("pe", lambda e, pg=pg, wg=wg, xT=xT, k=k, f=f: e.matmul(
                                pg[:], wg[:, k * DFF + f * 128:k * DFF + (f + 1) * 128], xT[:, k, :], start=(k == 0), stop=(k == 7)),
                                 reads=[wgres, xTres], writes=[pgres])
                        for k in range(8):
                            P.op("pe", lambda e, pu=pu, wu=wu, xT=xT, k=k, f=f: e.matmul(
                                pu[:], wu[:, k * DFF + f * 128:k * DFF + (f + 1) * 128], xT[:, k, :], start=(k == 0), stop=(k == 7)),
                                 reads=[wures, xTres], writes=[pures])
                        sg, sgres = sgr.get()
                        P.op("act", lambda e, sg=sg, pg=pg: e.activation(sg[:], pg[:], AF.Silu), reads=[pgres], writes=[sgres])
                        P.op("dve", lambda e, acT=acT, pu=pu, sg=sg, f=f: e.tensor_tensor(acT[:, f, :], pu[:], sg[:], ALU.mult),
                             reads=[pures, sgres], writes=[acres])
                    if i + 1 < 32:
                        do_transposes(i + 1)
                    for jj in range(4):
                        ys, ysres = ysr.get()
                        for half in range(2):
                            py, pyres = pyr.get()
                            for f in range(4):
                                P.op("pe", lambda e, py=py, acT=acT, wd=wd, f=f, jj=jj, half=half: e.matmul(
                                    py[:], acT[:, f, jj * 128:(jj + 1) * 128], wd[:, f, half * 512:(half + 1) * 512],
                                    start=(f == 0), stop=(f == 3)), reads=[acres, wdres], writes=[pyres])
                            if half == 0:
                                P.op("act", lambda e, ys=ys, py=py: e.copy(ys[:, 0:512], py[:]), reads=[pyres], writes=[ysres])
                            else:
                                P.op("dve", lambda e, ys=ys, py=py: e.tensor_copy(ys[:, 512:D], py[:]), reads=[pyres], writes=[ysres])
                        r0 = i * 512 + jj * 128
                        P.dma("act", y_d[r0:r0 + 128, :], ys[:], reads=[ysres], writes=[f"yd_{i}_{jj}"])
                    loaded.pop(i)
                    if i + 2 < 32:
                        issue_loads(i + 2)
                P.barrier()
                xcr = Ring(es, nc, "xcmb", [128, D], F32, 4)
                y1r = Ring(es, nc, "y1c", [128, D], F32, 3)
                y2r = Ring(es, nc, "y2c", [128, D], F32, 3)
                for t in range(32):
                    tok = t * 128
                    xt, xres = xcr.get()
                    P.dma("sp", xt[:], x1_d[tok:tok + 128, :], writes=[xres])
                    y1, y1res = y1r.get()
                    y2, y2res = y2r.get()
                    P.dma("pool", y1[:, :], y_d[:, :], ind=(None, POSI[:, 0, t:t + 1]), reads=["POSI"], writes=[y1res],
                          bounds_check=16383, oob_is_err=False)
                    P.dma("pool", y2[:, :], y_d[:, :], ind=(None, POSI[:, 1, t:t + 1]), reads=["POSI"], writes=[y2res],
                          bounds_check=16383, oob_is_err=False)
                    P.op("dve", lambda e, xt=xt, y1=y1, t=t: e.scalar_tensor_tensor(xt[:], y1[:], W12[:, 0, t:t + 1], xt[:],
                                                                                    ALU.mult, ALU.add),
                         reads=[xres, y1res, "W12"], writes=[xres])
                    P.op("dve", lambda e, xt=xt, y2=y2, t=t: e.scalar_tensor_tensor(xt[:], y2[:], W12[:, 1, t:t + 1], xt[:],
                                                                                    ALU.mult, ALU.add),
                         reads=[xres, y2res, "W12"], writes=[xres])
                    oid = P.dma("act", xout_d[tok:tok + 128, :], xt[:], reads=[xres], writes=[f"xout_{t}"])
                    final.append(oid)
                P.barrier()
        return final

    def layer1():
        phase_mods(1)
        with ExitStack() as esF:
            Ac = sb(esF, "Ac", [128, NT, D], BF16)
            As = sb(esF, "As", [128, NT, D], BF16)
            with ExitStack() as es:
                cc = sb(es, "cc", [128, 2, 2, 256], BF16)
                P.dma("sp", cc[:], cc_d, writes=["cc"])
                rings = dict(
                    junk=Ring(es, nc, "junkF", [128, D], BF16, 1),
                    stat=Ring(es, nc, "statF", [128, 4], F32, 4),
                    t1=Ring(es, nc, "t1F", [128, D], F32, 2),
                    pT=Ring(es, nc, "pTF", [128, D], BF16, 2, psum=True),
                )
                xr_ = Ring(es, nc, "xtF", [128, D], F32, 3)
                hbr = Ring(es, nc, "hbfF", [128, D], BF16, 4)
                hTr = Ring(es, nc, "hTF", [128, 8, 128], BF16, 3)
                par = Ring(es, nc, "paF", [128, 2, 512], F32, 3, psum=True)
                st1 = {}
                st2 = {}

                def f_stage1(t):
                    xt, xres = xr_.get()
                    P.dma("sp", xt[:], xmid_d[t * 128:(t + 1) * 128, :], writes=[xres])
                    hb, hbres = hbr.get()
                    norm_mod(rings, xt[:], xres, modsec("G1"), modsec("S1"), ["MOD0", "MOD1"], hb[:], hbres, "f", add_eng="dve")
                    st1[t] = (hb, hbres)

                def f_stage2(t):
                    hb, hbres = st1.pop(t)
                    hT, hTres = hTr.get()
                    transpose_tile(rings, hb, hbres, hT[:], hTres, identbf, BF16)
                    st2[t] = (hT, hTres)

                def f_stage3(t):
                    hT, hTres = st2.pop(t)
                    for cs in range(2):
                        pa, pares = par.get()
                        for g in range(4):
                            for kk in range(2):
                                P.op("pe", lambda e, pa=pa, hT=hT, cs=cs, g=g, kk=kk: e.matmul(
                                    pa[:, g // 2, (g % 2) * 256:(g % 2 + 1) * 256], hT[:, 2 * g + kk, :], cc[:, kk, cs, :],
                                    start=(kk == 0), stop=(kk == 1)), reads=[hTres, "cc"], writes=[pares])
                        A = Ac if cs == 0 else As
                        an = "Ac" if cs == 0 else "As"
                        P.op("act", lambda e, pa=pa, t=t, A=A: e.copy(A[:, t, 0:512], pa[:, 0, :]), reads=[pares], writes=[an])
                        P.op("dve", lambda e, pa=pa, t=t, A=A: e.tensor_copy(A[:, t, 512:D], pa[:, 1, :]), reads=[pares], writes=[an])

                f_stage1(0)
                f_stage1(1)
                f_stage2(0)
                for t in range(NT):
                    if t + 2 < NT:
                        f_stage1(t + 2)
                    if t + 1 < NT:
                        f_stage2(t + 1)
                    f_stage3(t)
                P.barrier()
            with ExitStack() as es:
                dfr = Ring(es, nc, "dfp", [128, 2, 512], BF16, 4)
                pbr = Ring(es, nc, "pbF", [128, 512], F32, 8, psum=True)
                fsr = Ring(es, nc, "fst", [128, 512], BF16, 4)
                qsr = Ring(es, nc, "qsF", [128, 512], F32, 2)
                for g8 in range(4):
                    for nq in range(4):
                        Pb = [pbr.get() for _ in range(2)]
                        Qb = [pbr.get() for _ in range(2)]
                        for nk in range(NT):
                            df, dfres = dfr.get()
                            P.dma("sp", df[:], dftn_d[nq, nk], writes=[dfres])
                            for f2 in range(2):
                                col = g8 * 256 + f2 * 128
                                P.op("pe", lambda e, pb=Pb[f2][0], nk=nk, col=col, df=df: e.matmul(
                                    pb[:], Ac[:, nk, col:col + 128], df[:, 0, :], start=(nk == 0), stop=(nk == NT - 1)),
                                     reads=[dfres, "Ac"], writes=[Pb[f2][1]])
                                P.op("pe", lambda e, qb=Qb[f2][0], nk=nk, col=col, df=df: e.matmul(
                                    qb[:], As[:, nk, col:col + 128], df[:, 1, :], start=(nk == 0), stop=(nk == NT - 1)),
                                     reads=[dfres, "As"], writes=[Qb[f2][1]])
                        for f2 in range(2):
                            row = g8 * 256 + f2 * 128
                            qs, qsres = qsr.get()
                            P.op("act", lambda e, qs=qs, qb=Qb[f2][0]: e.copy(qs[:], qb[:]), reads=[Qb[f2][1]], writes=[qsres])
                            fa, fares = fsr.get()
                            P.op("dve", lambda e, fa=fa, pb=Pb[f2][0], qs=qs: e.tensor_tensor(fa[:], pb[:], qs[:], ALU.add),
                                 reads=[Pb[f2][1], qsres], writes=[fares])
                            P.dma("act", cat_d[1][row:row + 128, 1 + nq * 512:1 + (nq + 1) * 512], fa[:], reads=[fares],
                                  writes=[f"cat1a_{g8}_{nq}_{f2}"])
                            fb, fbres = fsr.get()
                            P.op("dve", lambda e, fb=fb, pb=Pb[f2][0], qs=qs: e.tensor_tensor(fb[:, ::-1], pb[:], qs[:], ALU.subtract),
                                 reads=[Pb[f2][1], qsres], writes=[fbres])
                            c0 = 3584 - 512 * nq
                            P.dma("act", cat_d[1][row:row + 128, c0:c0 + 512], fb[:], reads=[fbres],
                                  writes=[f"cat1b_{g8}_{nq}_{f2}"])
                p0, p0res = pbr.get()
                for k in range(8):
                    for nk in range(NT):
                        P.op("pe", lambda e, k=k, nk=nk: e.matmul(p0[:, k:k + 1], Ac[:, nk, k * 128:(k + 1) * 128], onesbf[:, 0:1],
                                                                   start=(nk == 0), stop=(nk == NT - 1)),
                             reads=["Ac", "onesbf"], writes=[p0res])
                f0 = sb(es, "f0col", [128, 8], BF16)
                P.op("dve", lambda e: e.tensor_copy(f0[:], p0[:, 0:8]), reads=[p0res], writes=["f0col"])
                P.dma("sp", cat_d[1][:, 0:1].rearrange("(k p) o -> p (k o)", p=128), f0[:], reads=["f0col"], writes=["cat1_col0"],
                      allow_slow_non_contiguous=True)
                P.barrier()
        return phase_E(1, cat_d[1], fw_d, xmid_d, out_d)

    fin = layer0()
    if LAYERS > 1:
        fin = layer1()
    P.emit(final_wait_ops=fin)
    top.close()
    return nc, P


_CONST = {}


def _consts():
    if _CONST:
        return _CONST
    bf = ml_dtypes.bfloat16
    p = np.arange(128)
    cp = np.arange(256)
    cc = np.zeros((128, 2, 2, 256), np.float64)
    for kk in range(2):
        ang = 2 * np.pi * (((kk * 128 + p)[:, None] * cp[None, :]) % 256) / 256.0
        cc[:, kk, 0, :] = np.cos(ang) / 1024.0
        cc[:, kk, 1, :] = np.sin(ang) / 1024.0
    _CONST["dft_c"] = cc.astype(np.float32).astype(bf)
    n = np.arange(N, dtype=np.int64)
    m = (n[:, None] * n[None, :]) % N
    tab = np.cos(2 * np.pi * np.arange(N) / N).astype(np.float32)
    tabs = (-np.sin(2 * np.pi * np.arange(N) / N)).astype(np.float32)
    Cn = tab[m].astype(bf)
    Sn = tabs[m].astype(bf)
    arr = np.empty((4, 32, 128, 2, 512), bf)
    Cr = Cn[:, 1:2049].reshape(32, 128, 4, 512)
    Sr = Sn[:, 1:2049].reshape(32, 128, 4, 512)
    arr[:, :, :, 0, :] = Cr.transpose(2, 0, 1, 3)
    arr[:, :, :, 1, :] = Sr.transpose(2, 0, 1, 3)
    _CONST["dft_n"] = np.ascontiguousarray(arr)
    return _CONST


def _layout_inputs(inp):
    f32 = np.float32
    shared = {}
    shared["c_ctx"] = np.ascontiguousarray(inp["c_ctx"].reshape(8, 128), f32)
    for k in ("ada_w", "ada_b", "norm_mix", "norm_ffn", "router_w"):
        shared[k] = np.ascontiguousarray(inp[k], f32)
    shared["moe_w_gate"] = np.ascontiguousarray(inp["moe_w_gate"], f32).reshape(2 * NE * 128, 8 * DFF)
    shared["moe_w_up"] = np.ascontiguousarray(inp["moe_w_up"], f32).reshape(2 * NE * 128, 8 * DFF)
    shared["moe_w_down"] = np.ascontiguousarray(inp["moe_w_down"], f32).reshape(2 * NE * DFF, D)
    shared["mix_w_in"] = np.ascontiguousarray(inp["mix_w_in"][0], f32)
    shared["mix_w_out"] = np.ascontiguousarray(inp["mix_w_out"][0], f32)
    shared["fnet_w_out"] = np.ascontiguousarray(inp["fnet_w_out"][0], f32)
    shared["router_bias"] = np.ascontiguousarray(inp["router_bias"].reshape(1, NE), f32)
    qk = np.stack([np.tile(inp["na_q_norm"][0], 2), np.tile(inp["na_k_norm"][0], 2)], axis=1)
    shared["qk_gain"] = np.ascontiguousarray(qk, f32)
    rpb = np.asarray(inp["na_rpb"][0], f32)
    kc = np.arange(64)
    cq = np.arange(64)
    cs = np.clip(cq - 8, 0, 48)
    valid = (kc[:, None] >= cs[None, :]) & (kc[:, None] < cs[None, :] + 16)
    dc = np.clip(kc[:, None] - cq[None, :], -15, 15) + 15
    bt = np.empty((4, 128, 2, 14, 64), f32)
    for pr in range(4):
        for h in range(2):
            for half in range(2):
                for ds in range(14):
                    g = rpb[2 * pr + h, ds + half][dc]
                    bt[pr, half * 64:(half + 1) * 64, h, ds, :] = np.where(valid, g, f32(MASKV))
    shared["bt"] = bt
    cw = np.asarray(inp["lru_conv_w"][0], f32)
    shared["lru_cw"] = np.ascontiguousarray(cw.reshape(4, 4, 128).transpose(2, 1, 0))
    shared["lru_cb"] = np.ascontiguousarray(np.asarray(inp["lru_conv_b"][0], f32).reshape(4, 128).T)
    wbd = np.zeros((128, 16, 128), f32)
    gbias = np.empty((128, 16), f32)
    for gi, (wk, bk) in enumerate((("lru_gate_r_w", "lru_gate_r_b"), ("lru_gate_i_w", "lru_gate_i_b"))):
        w = np.asarray(inp[wk][0], f32)
        b = np.asarray(inp[bk][0], f32)
        for d in range(2):
            for c in range(4):
                idx = (gi * 2 + d) * 4 + c
                wbd[0:64, idx, 0:64] = w[d, 2 * c]
                wbd[64:128, idx, 64:128] = w[d, 2 * c + 1]
                gbias[:, idx] = b[d, c * 128:(c + 1) * 128]
    shared["lru_wbd"] = wbd
    shared["lru_gbias"] = gbias
    lam = np.asarray(inp["lru_lambda"][0], f32)
    shared["lru_lam"] = np.ascontiguousarray(lam.reshape(2, 4, 128).transpose(2, 0, 1).reshape(128, 8))
    shared.update(_consts())
    maps = []
    for b in range(8):
        m = dict(shared)
        m["x"] = np.ascontiguousarray(inp["x"][b], f32)
        m["c"] = np.ascontiguousarray(inp["c"][b].reshape(8, 128), f32)
        m["ctx"] = np.ascontiguousarray(inp["ctx"][b], f32)
        maps.append(m)
    return maps


_PROG = {}


def kernel(**inputs):
    inp = {k: np.asarray(v) for k, v in inputs.items()}
    maps = _layout_inputs(inp)
    if "nc" not in _PROG:
        _PROG["nc"], _PROG["P"] = build_program()
    nc = _PROG["nc"]
    ncores = int(os.environ.get("MK_CORES", "8"))
    res = run_bass_kernel_spmd(nc, maps[:ncores], core_ids=list(range(ncores)))
    if DEBUG:
        _PROG["res"] = res
    out = np.zeros((8, N, D), np.float32)
    for b in range(ncores):
        out[b] = res.results[b]["out"]
    return out
```

```python
import os
import numpy as np
import ml_dtypes
import concourse.bass as bass
import concourse.mybir as mybir
from concourse.bass_utils import run_bass_kernel_spmd
from contextlib import ExitStack

F32 = mybir.dt.float32
BF16 = mybir.dt.bfloat16
I32 = mybir.dt.int32
AF = mybir.ActivationFunctionType
ALU = mybir.AluOpType

D = 1024
N = 4096
CT = 256
NT = N // 128
NE = 16
DFF = 512
EPS = 1e-6
MASKV = -30000.0
DEBUG = bool(int(os.environ.get("MK_DEBUG", "0")))
LAYERS = int(os.environ.get("MK_LAYERS", "2"))

COMPUTE = ("pe", "act", "dve", "pool")


class Prog:
    def __init__(self, nc, n_dma_sems=12):
        self.nc = nc
        self.ops = []
        self.last_writer = {}
        self.readers = {}
        self.eng_ops = {e: [] for e in COMPUTE + ("sp",)}
        self.n_dma_sems = n_dma_sems
        self.dma_rr = {"sp": 0, "pool": 0, "act": 0}
        self.dma_cnt = {}
        self.dma_last = {}
        self.pending_barrier = {}

    def _deps(self, reads, writes):
        deps = set()
        for r in reads:
            w = self.last_writer.get(r)
            if w is not None:
                deps.add(w)
        for r in writes:
            w = self.last_writer.get(r)
            if w is not None:
                deps.add(w)
            for rd in self.readers.get(r, ()):
                deps.add(rd)
        return deps

    def _commit(self, oid, reads, writes):
        for r in reads:
            self.readers.setdefault(r, []).append(oid)
        for r in writes:
            self.last_writer[r] = oid
            self.readers[r] = []

    def op(self, eng, fn, reads=(), writes=()):
        oid = len(self.ops)
        deps = self._deps(reads, writes)
        pb = self.pending_barrier.pop(eng, None)
        if pb:
            deps |= pb
        o = dict(id=oid, eng=eng, fn=fn, deps=deps, dma=None, pos=len(self.eng_ops[eng]))
        self.ops.append(o)
        self.eng_ops[eng].append(oid)
        self._commit(oid, reads, writes)
        return oid

    def dma(self, q, out, in_, reads=(), writes=(), ind=None, **kw):
        oid = len(self.ops)
        deps = self._deps(reads, writes)
        pb = self.pending_barrier.pop(q, None)
        if pb:
            deps |= pb
        j = self.dma_rr[q]
        self.dma_rr[q] = (j + 1) % self.n_dma_sems
        chan = ("dma", q, j)
        prev = self.dma_last.get(chan)
        if prev is not None:
            deps.add(prev)
        self.dma_cnt[chan] = self.dma_cnt.get(chan, 0) + 16
        self.dma_last[chan] = oid
        o = dict(id=oid, eng=q, fn=None, deps=deps, dma=(out, in_, kw), ind=ind, chan=chan,
                 val=self.dma_cnt[chan], pos=len(self.eng_ops[q]))
        self.ops.append(o)
        self.eng_ops[q].append(oid)
        self._commit(oid, reads, writes)
        return oid

    def barrier(self):
        deps = set()
        for e, lst in self.eng_ops.items():
            if lst:
                deps.add(lst[-1])
        for chan, oid in self.dma_last.items():
            deps.add(oid)
        for e in self.eng_ops:
            self.pending_barrier[e] = set(deps) | self.pending_barrier.get(e, set())

    def emit(self, final_wait_ops=()):
        nc = self.nc
        ops = self.ops

        def chan_of(o):
            return o["chan"] if o["dma"] is not None else ("eng", o["eng"])

        def pos_of(o):
            return o["val"] if o["dma"] is not None else o["pos"] + 1

        clocks = {e: {} for e in self.eng_ops}
        waits = {}
        signals = set()
        snap = {}
        for o in ops:
            e = o["eng"]
            clk = clocks[e]
            need = {}
            for d in o["deps"]:
                p = ops[d]
                c = chan_of(p)
                v = pos_of(p)
                if p["dma"] is None and p["eng"] == e and e in ("pe", "sp"):
                    continue
                if clk.get(c, 0) >= v:
                    continue
                if c not in need or need[c][0] < v:
                    need[c] = (v, d)
            wl = []
            for c, (v, d) in need.items():
                if clk.get(c, 0) >= v:
                    continue
                wl.append(d)
                p = ops[d]
                if p["dma"] is None:
                    signals.add(d)
                for cc, vv in snap[d].items():
                    if clk.get(cc, 0) < vv:
                        clk[cc] = vv
                if clk.get(c, 0) < v:
                    clk[c] = v
            waits[o["id"]] = wl
            snap[o["id"]] = dict(clk)
        for d in final_wait_ops:
            if ops[d]["dma"] is None:
                signals.add(d)
        count_at = {}
        for e in COMPUTE:
            n = 0
            for oid in self.eng_ops[e]:
                if ops[oid]["dma"] is None and oid in signals:
                    n += 1
                    count_at[oid] = n
        self.stats = dict(n_ops=len(ops), n_signals=len(signals),
                          n_waits=sum(len(w) for w in waits.values()),
                          per_eng={e: len(v) for e, v in self.eng_ops.items()})
        es = ExitStack()
        sems = {}
        for e in COMPUTE:
            sems[("eng", e)] = es.enter_context(nc.semaphore(f"s_{e}"))
        for chan in self.dma_cnt:
            sems[chan] = es.enter_context(nc.semaphore(f"s_{chan[1]}_{chan[2]}"))
        block = es.enter_context(nc.Block())

        def wait_val(d):
            p = ops[d]
            return (sems[chan_of(p)], p["val"] if p["dma"] is not None else count_at[d])

        def make(e):
            def body(eng):
                bc_regs = {}
                for oid in self.eng_ops[e]:
                    o = ops[oid]
                    for d in waits[oid]:
                        s, v = wait_val(d)
                        eng.wait_ge(s, v)
                    if o["dma"] is not None:
                        out, in_, kw = o["dma"]
                        if o.get("ind") is not None:
                            oo, io = o["ind"]
                            if isinstance(kw.get("bounds_check"), int):
                                bc = kw["bounds_check"]
                                if bc not in bc_regs:
                                    bc_regs[bc] = eng.to_reg(bc)
                                kw = dict(kw, bounds_check=bc_regs[bc])
                            eng.indirect_dma_start(
                                out=out, out_offset=(None if oo is None else bass.IndirectOffsetOnAxis(ap=oo, axis=0)),
                                in_=in_, in_offset=(None if io is None else bass.IndirectOffsetOnAxis(ap=io, axis=0)),
                                **kw).then_inc(sems[o["chan"]], 16)
                        else:
                            eng.dma_start(out=out, in_=in_, **kw).then_inc(sems[o["chan"]], 16)
                    else:
                        ins = o["fn"](eng)
                        if oid in signals:
                            ins.then_inc(sems[("eng", e)], 1)
                if e == "sp":
                    for d in final_wait_ops:
                        s, v = wait_val(d)
                        eng.wait_ge(s, v)
            return body

        block.tensor(make("pe"))
        block.scalar(make("act"))
        block.vector(make("dve"))
        block.gpsimd(make("pool"))
        block.sync(make("sp"))
        es.close()


class Ring:
    uid = 0

    def __init__(self, es, nc, name, shape, dt, n, psum=False):
        self.name = name
        self.n = n
        self.i = 0
        alloc = nc.psum_tensor if psum else nc.sbuf_tensor
        Ring.uid += 1
        self.bufs = [es.enter_context(alloc(f"{name}{j}_r{Ring.uid}", shape, dt)) for j in range(n)]
        self.name = f"{name}_r{Ring.uid}_"

    def get(self):
        j = self.i % self.n
        self.i += 1
        return self.bufs[j], f"{self.name}{j}"


def build_program():
    nc = bass.Bass("TRN2", target_bir_lowering=False)
    P = Prog(nc, n_dma_sems=12)

    def din(name, shape, dt=F32):
        return nc.dram_tensor(name, list(shape), dt, kind="ExternalInput").ap()

    def dscr(name, shape, dt, dbg=False):
        kind = "ExternalOutput" if (dbg and DEBUG) else "Internal"
        return nc.dram_tensor(name, list(shape), dt, kind=kind).ap()

    x_d = din("x", [N, D])
    c_d = din("c", [8, 128])
    ctx_d = din("ctx", [CT, D])
    cctx_d = din("c_ctx", [8, 128])
    adaw_d = din("ada_w", [2, D, 6 * D])
    adab_d = din("ada_b", [2, 6 * D])
    nmix_d = din("norm_mix", [2, D])
    nffn_d = din("norm_ffn", [2, D])
    win_d = din("mix_w_in", [D, 2560])
    wout_d = din("mix_w_out", [D, D])
    qkg_d = din("qk_gain", [128, 2])
    bt_d = din("bt", [4, 128, 2, 14, 64])
    cw_d = din("lru_cw", [128, 4, 4])
    cb_d = din("lru_cb", [128, 4])
    wbd_d = din("lru_wbd", [128, 16, 128])
    gbias_d = din("lru_gbias", [128, 16])
    lam_d = din("lru_lam", [128, 8])
    fw_d = din("fnet_w_out", [D, D])
    rw_d = din("router_w", [D, NE])
    rb_d = din("router_bias", [1, NE])
    wg_d = din("moe_w_gate", [2 * NE * 128, 8 * DFF])
    wu_d = din("moe_w_up", [2 * NE * 128, 8 * DFF])
    wd_d = din("moe_w_down", [2 * NE * DFF, D])
    cc_d = din("dft_c", [128, 2, 2, 256], BF16)
    dftn_d = din("dft_n", [4, 32, 128, 2, 512], BF16)
    out_d = nc.dram_tensor("out", [N, D], F32, kind="ExternalOutput").ap()

    qT_d = dscr("qT_s", [512, N], BF16, True)
    kT_d = dscr("kT_s", [512, N + CT], BF16, True)
    v_d = dscr("v_s", [N + CT, 512], BF16, True)
    xbT_d = dscr("xbT_s", [512, N + CT], F32, True)
    gbT_d = dscr("gbT_s", [512, N], F32, True)
    cat_d = [dscr("cat0_s", [D, N], BF16, True), dscr("cat1_s", [D, N], BF16, True)]
    xmid_d = dscr("xmid_s", [N, D], F32, True)
    x1_d = dscr("x1_s", [N, D], F32)
    xs_d = dscr("xs_s", [16384, D], BF16)
    y_d = dscr("y_s", [16384, D], F32)

    top = ExitStack()

    def sb(es, name, shape, dt):
        Ring.uid += 1
        return es.enter_context(nc.sbuf_tensor(f"{name}_u{Ring.uid}", list(shape), dt))

    def psm(es, name, shape, dt):
        Ring.uid += 1
        return es.enter_context(nc.psum_tensor(f"{name}_u{Ring.uid}", list(shape), dt))

    ident32 = sb(top, "ident32", [128, 128], F32)
    identbf = sb(top, "identbf", [128, 128], BF16)
    ones32 = sb(top, "ones32", [128, 128], F32)
    onesbf = sb(top, "onesbf", [128, 128], BF16)
    blk32 = sb(top, "blk32", [128, 128], F32)
    blkbf = sb(top, "blkbf", [128, 128], BF16)
    cbT = sb(top, "cbT", [128, 8, 128], F32)
    MOD = sb(top, "MOD", [128, 6 * D], F32)
    rw32 = sb(top, "rw32", [128, 8, NE], F32)
    rbias = sb(top, "rbias", [128, NE], F32)
    cols = sb(top, "cols", [128, 8], F32)

    P.op("pool", lambda e: e.memset(ident32[:], 0.0), writes=["ident32"])
    P.op("pool", lambda e: e.affine_select(ident32[:], ident32[:], [[-1, 128]], ALU.not_equal, 1.0,
                                           base=0, channel_multiplier=1), reads=["ident32"], writes=["ident32"])
    P.op("dve", lambda e: e.tensor_copy(identbf[:], ident32[:]), reads=["ident32"], writes=["identbf"])
    P.op("pool", lambda e: e.memset(ones32[:], 1.0), writes=["ones32"])
    P.op("pool", lambda e: e.memset(onesbf[:], 1.0), writes=["onesbf"])
    P.op("pool", lambda e: e.memset(blk32[:], 0.0), writes=["blk32"])
    P.op("pool", lambda e: e.memset(blk32[0:64, 0:64], 1.0), reads=["blk32"], writes=["blk32"])
    P.op("pool", lambda e: e.memset(blk32[64:128, 64:128], 1.0), reads=["blk32"], writes=["blk32"])
    P.op("dve", lambda e: e.tensor_copy(blkbf[:], blk32[:]), reads=["blk32"], writes=["blkbf"])
    P.op("pool", lambda e: e.memset(cols[:, 0:1], EPS), writes=["cols"])
    P.op("pool", lambda e: e.memset(cols[:, 1:2], 64 * EPS), reads=["cols"], writes=["cols"])
    P.op("pool", lambda e: e.memset(cols[:, 2:3], 1.0), reads=["cols"], writes=["cols"])
    P.dma("sp", rw32[:], rw_d.rearrange("(k p) e -> p k e", p=128), writes=["rw32"])
    P.dma("sp", rbias[:], rb_d.partition_broadcast(128), writes=["rbias"])

    SEC = {"S1": 0, "G1": 1, "GATE1": 2, "S2": 3, "G2": 4, "GATE2": 5}

    def modsec(name):
        j = SEC[name]
        return MOD[:, j * D:(j + 1) * D]

    def make_cb(es, src_d, dst, tag):
        crow = sb(es, f"crow{tag}", [8, 128], F32)
        ccol = sb(es, f"ccol{tag}", [128, 8], F32)
        pt = psm(es, f"cps{tag}", [128, 8], F32)
        P.dma("sp", crow[:], src_d, writes=[f"crow{tag}"])
        P.op("act", lambda e: e.activation(crow[:], crow[:], AF.Silu), reads=[f"crow{tag}"], writes=[f"crow{tag}"])
        P.op("pe", lambda e: e.transpose(pt[:], crow[:], ident32[0:8, 0:8]), reads=[f"crow{tag}", "ident32"],
             writes=[f"cps{tag}"])
        P.op("dve", lambda e: e.tensor_copy(ccol[:], pt[:]), reads=[f"cps{tag}"], writes=[f"ccol{tag}"])
        for k in range(8):
            P.op("dve", lambda e, k=k: e.tensor_scalar(dst[:, k, :], ones32[:], ccol[:, k:k + 1], None, ALU.mult),
                 reads=[f"ccol{tag}", "ones32"], writes=[f"cb{tag}"])

    with ExitStack() as es0:
        make_cb(es0, c_d, cbT, "c")
        P.barrier()

    def phase_mods(l, MODC=None, cxT=None):
        with ExitStack() as es:
            abias = sb(es, "abias", [128, 6 * D], F32)
            gm = sb(es, "gm", [128, D], F32)
            gf = sb(es, "gf", [128, D], F32)
            awr = Ring(es, nc, "aw", [128, 8, 512], F32, 2)
            psr = Ring(es, nc, "modps", [128, 512], F32, 2, psum=True)
            psc = Ring(es, nc, "modpc", [128, 512], F32, 2, psum=True)
            P.dma("sp", abias[:], adab_d[l:l + 1, :].partition_broadcast(128), writes=["abias"])
            P.dma("sp", gm[:], nmix_d[l:l + 1, :].partition_broadcast(128), writes=["gm"])
            P.dma("sp", gf[:], nffn_d[l:l + 1, :].partition_broadcast(128), writes=["gf"])
            for j in range(12):
                aw, ar = awr.get()
                P.dma("sp", aw[:], adaw_d[l][:, j * 512:(j + 1) * 512].rearrange("(k p) n -> p k n", p=128),
                      writes=[ar])
                ps, pr = psr.get()
                for k in range(8):
                    P.op("pe", lambda e, ps=ps, aw=aw, k=k: e.matmul(ps[:], cbT[:, k, :], aw[:, k, :],
                                                                      start=(k == 0), stop=(k == 7)),
                         reads=[ar, "cbc"], writes=[pr])
                P.op("dve", lambda e, ps=ps, j=j: e.tensor_tensor(MOD[:, j * 512:(j + 1) * 512], ps[:],
                                                                   abias[:, j * 512:(j + 1) * 512], ALU.add),
                     reads=[pr, "abias"], writes=[f"MOD{j // 2}"])
                if MODC is not None and j < 4:
                    pc, pcr = psc.get()
                    for k in range(8):
                        P.op("pe", lambda e, pc=pc, aw=aw, k=k: e.matmul(pc[:], cxT[:, k, :], aw[:, k, :],
                                                                          start=(k == 0), stop=(k == 7)),
                             reads=[ar, "cbx"], writes=[pcr])
                    P.op("dve", lambda e, pc=pc, j=j: e.tensor_tensor(MODC[:, j * 512:(j + 1) * 512], pc[:],
                                                                       abias[:, j * 512:(j + 1) * 512], ALU.add),
                         reads=[pcr, "abias"], writes=[f"MODC{j // 2}"])
            P.op("dve", lambda e: e.scalar_tensor_tensor(modsec("G1"), modsec("G1"), 1.0, gm[:], ALU.add, ALU.mult),
                 reads=["MOD1", "gm"], writes=["MOD1"])
            P.op("dve", lambda e: e.scalar_tensor_tensor(modsec("G2"), modsec("G2"), 1.0, gf[:], ALU.add, ALU.mult),
                 reads=["MOD4", "gf"], writes=["MOD4"])
            if MODC is not None:
                P.op("dve", lambda e: e.scalar_tensor_tensor(MODC[:, D:2 * D], MODC[:, D:2 * D], 1.0, gm[:],
                                                             ALU.add, ALU.mult),
                     reads=["MODC1", "gm"], writes=["MODC1"])
            P.barrier()

    def norm_mod(es_rings, xt, xr, G, S, gres, out, outr, tag, add_eng="pool"):
        junk, jr = es_rings["junk"].get()
        st, sr = es_rings["stat"].get()
        t1, t1r = es_rings["t1"].get()
        P.op("act", lambda e: e.activation(junk[:], xt, AF.Square, accum_out=st[:, 0:1]),
             reads=[xr], writes=[jr, sr])
        P.op("act", lambda e: e.activation(st[:, 1:2], st[:, 0:1], AF.Ln, bias=cols[:, 0:1], scale=1.0 / D),
             reads=[sr, "cols"], writes=[sr])
        P.op("act", lambda e: e.activation(st[:, 2:3], st[:, 1:2], AF.Exp, scale=-0.5), reads=[sr], writes=[sr])
        P.op("dve", lambda e: e.scalar_tensor_tensor(t1[:], xt, st[:, 2:3], G, ALU.mult, ALU.mult),
             reads=[xr, sr] + gres, writes=[t1r])
        P.op(add_eng, lambda e: e.tensor_tensor(out, t1[:], S, ALU.add), reads=[t1r] + gres, writes=[outr])

    def transpose_tile(es_rings, src_tile, srcr, dst_ap, dstr, ident, dt):
        pt, ptr = es_rings["pT"].get()
        for k in range(8):
            P.op("pe", lambda e, k=k: e.transpose(pt[:, k * 128:(k + 1) * 128], src_tile[:, k * 128:(k + 1) * 128],
                                                    ident[:]),
                 reads=[srcr, "identbf", "ident32"], writes=[ptr])
        P.op("act", lambda e: e.copy(dst_ap, pt[:].rearrange("p (k t) -> p k t", k=8)), reads=[ptr], writes=[dstr])

    def layer0():
        with ExitStack() as esL:
          with ExitStack() as esAB:
            MODC = sb(esAB, "MODC", [128, 2 * D], F32)
            with ExitStack() as esA:
                cxT = sb(esA, "cxT", [128, 8, 128], F32)
                make_cb(esA, cctx_d, cxT, "x")
                phase_mods(0, MODC, cxT)
            with ExitStack() as es:
                win = sb(es, "win", [128, 8, 2560], BF16)
                qkg = sb(es, "qkg", [128, 2], F32)
                for k in range(8):
                    P.dma("pool", win[:, k, :], win_d[k * 128:(k + 1) * 128, :], writes=["win"])
                P.dma("sp", qkg[:], qkg_d, writes=["qkg"])
                rings = dict(
                    junk=Ring(es, nc, "junk", [128, D], F32, 1),
                    stat=Ring(es, nc, "stat", [128, 4], F32, 4),
                    t1=Ring(es, nc, "t1", [128, D], F32, 2),
                    pT=Ring(es, nc, "pT", [128, D], BF16, 2, psum=True),
                )
                xr_ = Ring(es, nc, "xt", [128, D], F32, 4)
                hbr = Ring(es, nc, "hbf", [128, D], BF16, 5)
                hTr = Ring(es, nc, "hT", [128, 8, 512], BF16, 3)
                ppr = Ring(es, nc, "pp", [128, 512], F32, 4, psum=True)
                ssr = Ring(es, nc, "ssp", [128, 512], F32, 2, psum=True)
                sqr = Ring(es, nc, "sq", [128, 512], BF16, 2)
                rsr = Ring(es, nc, "rs", [128, 512], F32, 2)
                rdr = Ring(es, nc, "rd", [128, 512], F32, 2)
                qor = Ring(es, nc, "qo", [128, 512], BF16, 3)
                stg = Ring(es, nc, "stg", [128, 512], F32, 3)
                vor = Ring(es, nc, "vo", [128, 512], BF16, 2)

                def proj_prep(src_d, tok0, ntok, G, S, gres):
                    nt = ntok // 128
                    hT, hTres = hTr.get()
                    hbs = []
                    for t in range(nt):
                        xt, xres = xr_.get()
                        P.dma("act", xt[:], src_d[tok0 + t * 128: tok0 + (t + 1) * 128, :], writes=[xres])
                        hb, hbres = hbr.get()
                        norm_mod(rings, xt[:], xres, G, S, gres, hb[:], hbres, "b", add_eng="dve")
                        hbs.append((hb, hbres))
                    for t in range(nt):
                        hb, hbres = hbs[t]
                        transpose_tile(rings, hb, hbres, hT[:, :, t * 128:(t + 1) * 128], hTres, identbf, BF16)
                    return hT, hTres

                def proj_compute(hT, hTres, ntok, is_ctx, col0):
                    nt = ntok // 128
                    fm = []
                    if not is_ctx:
                        fm += [("q", c) for c in range(4)]
                    fm += [("k", c) for c in range(4)]
                    fm += [("xb", c) for c in range(4)]
                    if not is_ctx:
                        fm += [("gb", c) for c in range(4)]
                    base = {"q": 0, "k": 512, "xb": 1536, "gb": 2048}
                    pending = None
                    for (kind, c) in fm:
                        wc = base[kind] + c * 128
                        pp, ppres = ppr.get()
                        for k in range(8):
                            P.op("pe", lambda e, pp=pp, k=k, wc=wc: e.matmul(pp[:, 0:ntok], win[:, k, wc:wc + 128],
                                                                              hT[:, k, 0:ntok], start=(k == 0),
                                                                              stop=(k == 7)),
                                 reads=["win", hTres], writes=[ppres])
                        def post(kind=kind, c=c, pp=pp, ppres=ppres):
                            if kind in ("q", "k"):
                                sq, sqres = sqr.get()
                                P.op("act", lambda e, pp=pp, sq=sq: e.activation(sq[:, 0:ntok], pp[:, 0:ntok], AF.Square),
                                     reads=[ppres], writes=[sqres])
                                ssp, sspres = ssr.get()
                                P.op("pe", lambda e, ssp=ssp, sq=sq: e.matmul(ssp[:, 0:ntok], blkbf[:], sq[:, 0:ntok],
                                                                              start=True, stop=True),
                                     reads=[sqres, "blkbf"], writes=[sspres])
                                rs, rsres = rsr.get()
                                if kind == "q":
                                    P.op("act", lambda e, rs=rs, ssp=ssp: e.activation(rs[:, 0:ntok], ssp[:, 0:ntok], AF.Ln,
                                                                                       bias=cols[:, 1:2], scale=1.0),
                                         reads=[sspres, "cols"], writes=[rsres])
                                else:
                                    P.op("act", lambda e, rs=rs, ssp=ssp: e.activation(rs[:, 0:ntok], ssp[:, 0:ntok], AF.Ln,
                                                                                       bias=cols[:, 0:1], scale=1.0 / 64),
                                         reads=[sspres, "cols"], writes=[rsres])
                                rd, rdres = rdr.get()
                                P.op("act", lambda e, rd=rd, rs=rs: e.activation(rd[:, 0:ntok], rs[:, 0:ntok], AF.Exp, scale=-0.5),
                                     reads=[rsres], writes=[rdres])
                                qo, qores = qor.get()
                                gi = 0 if kind == "q" else 1
                                P.op("dve", lambda e, qo=qo, pp=pp, rd=rd, gi=gi: e.scalar_tensor_tensor(
                                    qo[:, 0:ntok], pp[:, 0:ntok], qkg[:, gi:gi + 1], rd[:, 0:ntok], ALU.mult, ALU.mult),
                                     reads=[ppres, rdres, "qkg"], writes=[qores])
                                dst = qT_d if kind == "q" else kT_d
                                P.dma("sp", dst[c * 128:(c + 1) * 128, col0:col0 + ntok], qo[:, 0:ntok],
                                      reads=[qores], writes=[f"{kind}T_d_{c}_{col0}"])
                            else:
                                sg, sgres = stg.get()
                                P.op("act", lambda e, sg=sg, pp=pp: e.copy(sg[:, 0:ntok], pp[:, 0:ntok]),
                                     reads=[ppres], writes=[sgres])
                                dst = xbT_d if kind == "xb" else gbT_d
                                P.dma("sp", dst[c * 128:(c + 1) * 128, col0:col0 + ntok], sg[:, 0:ntok],
                                      reads=[sgres], writes=[f"{kind}T_d_{c}_{col0}"])
                        if pending is not None:
                            pending()
                        pending = post
                    if pending is not None:
                        pending()
                    for t in range(nt):
                        pp, ppres = ppr.get()
                        for k in range(8):
                            P.op("pe", lambda e, pp=pp, k=k, t=t: e.matmul(pp[:], hT[:, k, t * 128:(t + 1) * 128],
                                                                            win[:, k, 1024:1536], start=(k == 0),
                                                                            stop=(k == 7)),
                                 reads=["win", hTres], writes=[ppres])
                        vo, vores = vor.get()
                        P.op("dve", lambda e, vo=vo, pp=pp: e.tensor_copy(vo[:], pp[:]), reads=[ppres], writes=[vores])
                        P.dma("sp", v_d[col0 + t * 128: col0 + (t + 1) * 128, :], vo[:], reads=[vores], writes=[f"v_d_{col0}_{t}"])

                preps = {}
                preps[-1] = proj_prep(ctx_d, 0, CT, MODC[:, D:2 * D], MODC[:, 0:D], ["MODC0", "MODC1"])
                preps[0] = proj_prep(x_d, 0, 512, modsec("G1"), modsec("S1"), ["MOD0", "MOD1"])
                for tc in range(-1, 8):
                    if tc + 2 < 8:
                        preps[tc + 2] = proj_prep(x_d, (tc + 2) * 512, 512, modsec("G1"), modsec("S1"), ["MOD0", "MOD1"])
                    cur = preps.pop(tc)
                    if tc < 0:
                        proj_compute(cur[0], cur[1], CT, True, N)
                    else:
                        proj_compute(cur[0], cur[1], 512, False, tc * 512)
                P.barrier()
          with ExitStack() as es:
              qTp = Ring(es, nc, "qTp", [128, N], BF16, 2)
              kTp = Ring(es, nc, "kTp", [128, N + CT], BF16, 2)
              Vp = Ring(es, nc, "Vp", [128, NT + 2, 128], BF16, 2)
              BTp = Ring(es, nc, "BTp", [128, 2, 14, 64], F32, 2)
              SPr = Ring(es, nc, "SP", [128, 2, 8, 64], F32, 3, psum=True)
              OTr = Ring(es, nc, "OT", [128, 256], F32, 2, psum=True)
              Sbr = Ring(es, nc, "Sb", [128, 2, 8, 64], F32, 3)
              PTr = Ring(es, nc, "PT", [128, 2, 8, 64], BF16, 3)
              rcr = Ring(es, nc, "rc", [128, 128], F32, 2)
              asr = Ring(es, nc, "ast", [128, 512], BF16, 3)
              units = []
              for p in range(4):
                  for r in range(64):
                      units.append(dict(p=p, r=r))
              pair_bufs = {}

              def load_pair(p):
                  qT, qres = qTp.get()
                  kT, kres = kTp.get()
                  V, vres = Vp.get()
                  BT, bres = BTp.get()
                  P.dma("sp", qT[:], qT_d[p * 128:(p + 1) * 128, :], writes=[qres])
                  P.dma("sp", kT[:], kT_d[p * 128:(p + 1) * 128, :], writes=[kres])
                  for t4 in range(0, NT + 2, 2):
                      P.dma("sp", V[:, t4:t4 + 2, :],
                            v_d[t4 * 128:(t4 + 2) * 128, p * 128:(p + 1) * 128].rearrange("(t q) c -> q t c", q=128),
                            writes=[vres])
                  P.dma("sp", BT[:], bt_d[p], writes=[bres])
                  pair_bufs[p] = (qT, qres, kT, kres, V, vres, BT, bres)

              def stageA(u):
                  p, r = u["p"], u["r"]
                  if p not in pair_bufs:
                      load_pair(p)
                  qT, qres, kT, kres, V, vres, BT, bres = pair_bufs[p]
                  rs_ = min(max(r - 4, 0), 56)
                  if rs_ % 2 == 0:
                      tile0, nslot, parts, d0 = rs_ // 2, 4, [(0, 128)] * 4, rs_ - r + 7
                  else:
                      tile0, nslot, d0 = (rs_ - 1) // 2, 5, rs_ - 1 - r + 7
                      parts = [(64, 128), (0, 128), (0, 128), (0, 128), (0, 64)]
                  SP, spres = SPr.get()
                  u.update(tile0=tile0, nslot=nslot, parts=parts, d0=d0, SP=SP, spres=spres)
                  qs = slice(r * 64, (r + 1) * 64)
                  for h in range(2):
                      hp = slice(h * 64, (h + 1) * 64)
                      for s in range(nslot + 2):
                          tk = (tile0 + s) * 128 if s < nslot else N + (s - nslot) * 128
                          P.op("pe", lambda e, SP=SP, h=h, s=s, hp=hp, tk=tk, kT=kT, qT=qT, qs=qs: e.matmul(
                              SP[:, h, s, :], kT[hp, tk:tk + 128], qT[hp, qs], start=True, stop=True),
                               reads=[kres, qres], writes=[spres])

              def stageBC(u):
                  qT, qres, kT, kres, V, vres, BT, bres = pair_bufs[u["p"]]
                  SP, spres, nslot, d0 = u["SP"], u["spres"], u["nslot"], u["d0"]
                  Sb, sbres = Sbr.get()
                  for h in range(2):
                      P.op("dve", lambda e, Sb=Sb, SP=SP, h=h, BT=BT, d0=d0, nslot=nslot: e.tensor_tensor(
                          Sb[:, h, 0:nslot, :], SP[:, h, 0:nslot, :], BT[:, h, d0:d0 + 2 * nslot - 1:2, :], ALU.add),
                           reads=[spres, bres], writes=[sbres])
                  PT, ptres = PTr.get()
                  P.op("act", lambda e, PT=PT, Sb=Sb, nslot=nslot: e.activation(PT[:, :, 0:nslot, :], Sb[:, :, 0:nslot, :], AF.Exp),
                       reads=[sbres], writes=[ptres])
                  for h in range(2):
                      P.op("act", lambda e, PT=PT, SP=SP, h=h, nslot=nslot: e.activation(
                          PT[:, h, nslot:nslot + 2, :], SP[:, h, nslot:nslot + 2, :], AF.Exp),
                           reads=[spres], writes=[ptres])
                  u.update(PT=PT, ptres=ptres)

              ast_state = {}

              def stageDE(u):
                  p, r = u["p"], u["r"]
                  qT, qres, kT, kres, V, vres, BT, bres = pair_bufs[p]
                  PT, ptres, nslot, parts, tile0 = u["PT"], u["ptres"], u["nslot"], u["parts"], u["tile0"]
                  OT, otres = OTr.get()
                  ns = nslot + 2
                  for which in range(2):
                      for s in range(ns):
                          if s < nslot:
                              lo, hi = parts[s]
                              vt = tile0 + s
                          else:
                              lo, hi = 0, 128
                              vt = NT + (s - nslot)
                          if which == 0:
                              P.op("pe", lambda e, OT=OT, PT=PT, s=s, lo=lo, hi=hi, vt=vt, V=V, ns=ns: e.matmul(
                                  OT[:, 0:128].rearrange("p (h q) -> p h q", h=2), V[lo:hi, vt, :],
                                  PT[lo:hi, :, s, :], start=(s == 0), stop=(s == ns - 1)),
                                   reads=[vres, ptres], writes=[otres])
                          else:
                              P.op("pe", lambda e, OT=OT, PT=PT, s=s, lo=lo, hi=hi, ns=ns: e.matmul(
                                  OT[:, 128:256].rearrange("p (h q) -> p h q", h=2), onesbf[lo:hi, :],
                                  PT[lo:hi, :, s, :], start=(s == 0), stop=(s == ns - 1)),
                                   reads=["onesbf", ptres], writes=[otres])
                  rc, rcres = rcr.get()
                  P.op("dve", lambda e, rc=rc, OT=OT: e.reciprocal(rc[:], OT[:, 128:256]), reads=[otres], writes=[rcres])
                  if r % 8 == 0:
                      ast_state["cur"] = asr.get()
                  ast, astres = ast_state["cur"]
                  rr = r % 8
                  for h in range(2):
                      hp = slice(h * 64, (h + 1) * 64)
                      P.op("dve", lambda e, ast=ast, OT=OT, rc=rc, hp=hp, rr=rr: e.tensor_tensor(
                          ast[hp, rr * 64:(rr + 1) * 64], OT[hp, hp], rc[hp, hp], ALU.mult),
                           reads=[otres, rcres], writes=[astres])
                  if rr == 7:
                      r0 = r - 7
                      P.dma("act", cat_d[0][p * 128:(p + 1) * 128, r0 * 64:(r0 + 8) * 64], ast[:],
                            reads=[astres], writes=[f"cat0a_{p}_{r}"])

              nu = len(units)
              for step in range(nu + 2):
                  if step < nu:
                      stageA(units[step])
                  if 0 <= step - 1 < nu:
                      stageBC(units[step - 1])
                  if 0 <= step - 2 < nu:
                      stageDE(units[step - 2])
              P.barrier()
          with ExitStack() as es:
              L = N + CT
              cw = sb(es, "cw", [128, 4, 4], F32)
              cbv = sb(es, "cbv", [128, 4], F32)
              wbd = sb(es, "wbd", [128, 16, 128], BF16)
              gbias = sb(es, "gbias", [128, 16], F32)
              lam = sb(es, "lam", [128, 8], F32)
              m8 = sb(es, "m8", [128, 8], F32)
              m16 = sb(es, "m16", [128, 8], F32)
              P.dma("sp", cw[:], cw_d, writes=["cw"])
              P.dma("sp", cbv[:], cb_d, writes=["cbv"])
              P.dma("pool", wbd[:], wbd_d, writes=["wbd"])
              P.dma("sp", gbias[:], gbias_d, writes=["gbias"])
              P.dma("sp", lam[:], lam_d, writes=["lam"])
              P.op("act", lambda e: e.activation(m8[:], lam[:], AF.Exp, scale=-1.0), reads=["lam"], writes=["m8"])
              P.op("act", lambda e: e.activation(m8[:], m8[:], AF.Ln, bias=cols[:, 2:3], scale=1.0),
                   reads=["m8", "cols"], writes=["m8"])
              P.op("dve", lambda e: e.tensor_scalar(m16[:], m8[:], -16.0, None, ALU.mult), reads=["m8"], writes=["m16"])
              P.op("dve", lambda e: e.tensor_scalar(m8[:], m8[:], -8.0, None, ALU.mult), reads=["m8", "m16"], writes=["m8"])
              xpad = sb(es, "xpad", [128, N + 4], F32)
              xpc = sb(es, "xpc", [128, CT + 4], F32)
              xc = sb(es, "xc", [128, L], F32)
              xcb = sb(es, "xcb", [128, L], BF16)
              rt = sb(es, "rt", [128, L], F32)
              it = sb(es, "it", [128, L], F32)
              at = sb(es, "at", [128, L], F32)
              hf = sb(es, "hf", [128, L], F32)
              hb_ = sb(es, "hb", [128, L], F32)
              gbt = sb(es, "gbt", [128, N], F32)
              lro = sb(es, "lro", [128, N], BF16)
              gpr = Ring(es, nc, "gps", [128, 512], F32, 4, psum=True)
              P.op("pool", lambda e: e.memset(xpad[:, 0:2], 0.0), writes=["xpad_l"])
              P.op("pool", lambda e: e.memset(xpad[:, N + 2:N + 4], 0.0), writes=["xpad_r"])
              P.op("pool", lambda e: e.memset(xpc[:, 0:2], 0.0), writes=["xpc_l"])
              P.op("pool", lambda e: e.memset(xpc[:, CT + 2:CT + 4], 0.0), writes=["xpc_r"])
              pieces = [(0, CT)] + [(CT + i * 512, 512) for i in range(8)]
              for c in range(4):
                  P.dma("sp", xpad[:, 2:N + 2], xbT_d[c * 128:(c + 1) * 128, 0:N], reads=["xbT_d"], writes=["xpad"])
                  P.dma("sp", xpc[:, 2:CT + 2], xbT_d[c * 128:(c + 1) * 128, N:N + CT], reads=["xbT_d"], writes=["xpc"])
                  P.dma("sp", gbt[:], gbT_d[c * 128:(c + 1) * 128, :], reads=["gbT_d"], writes=["gbt"])
                  for (src, srcres, n, o0) in ((xpc, ["xpc", "xpc_l", "xpc_r"], CT, 0),
                                               (xpad, ["xpad", "xpad_l", "xpad_r"], N, CT)):
                      P.op("dve", lambda e, src=src, n=n, o0=o0, c=c: e.tensor_scalar(
                          xc[:, o0:o0 + n], src[:, 0:n], cw[:, c, 0:1], cbv[:, c:c + 1], ALU.mult, ALU.add),
                           reads=srcres + ["cw", "cbv"], writes=["xc"])
                      for tap in range(1, 4):
                          P.op("dve", lambda e, src=src, n=n, o0=o0, c=c, tap=tap: e.scalar_tensor_tensor(
                              xc[:, o0:o0 + n], src[:, tap:tap + n], cw[:, c, tap:tap + 1], xc[:, o0:o0 + n],
                              ALU.mult, ALU.add), reads=srcres + ["cw", "xc"], writes=["xc"])
                  P.op("act", lambda e: e.copy(xcb[:], xc[:]), reads=["xc"], writes=["xcb"])
                  for d in range(2):
                      ir = (0 * 2 + d) * 4 + c
                      ii = (1 * 2 + d) * 4 + c
                      for (o0, n) in pieces:
                          g1, g1r = gpr.get()
                          P.op("pe", lambda e, g1=g1, o0=o0, n=n, ir=ir: e.matmul(g1[:, 0:n], wbd[:, ir, :], xcb[:, o0:o0 + n],
                                                                                  start=True, stop=True),
                               reads=["wbd", "xcb"], writes=[g1r])
                          P.op("act", lambda e, g1=g1, o0=o0, n=n, ir=ir: e.activation(
                              rt[:, o0:o0 + n], g1[:, 0:n], AF.Sigmoid, bias=gbias[:, ir:ir + 1], scale=1.0),
                               reads=[g1r, "gbias"], writes=["rt"])
                          g2, g2r = gpr.get()
                          P.op("pe", lambda e, g2=g2, o0=o0, n=n, ii=ii: e.matmul(g2[:, 0:n], wbd[:, ii, :], xcb[:, o0:o0 + n],
                                                                                  start=True, stop=True),
                               reads=["wbd", "xcb"], writes=[g2r])
                          P.op("act", lambda e, g2=g2, o0=o0, n=n, ii=ii: e.activation(
                              it[:, o0:o0 + n], g2[:, 0:n], AF.Sigmoid, bias=gbias[:, ii:ii + 1], scale=1.0),
                               reads=[g2r, "gbias"], writes=["it"])
                      lc = d * 4 + c
                      P.op("act", lambda e, lc=lc: e.activation(at[:], rt[:], AF.Exp, scale=m8[:, lc:lc + 1]),
                           reads=["rt", "m8"], writes=["at"])
                      P.op("act", lambda e, lc=lc: e.activation(rt[:], rt[:], AF.Exp, scale=m16[:, lc:lc + 1]),
                           reads=["rt", "m16"], writes=["rt"])
                      P.op("act", lambda e: e.activation(rt[:], rt[:], AF.Sqrt, bias=cols[:, 2:3], scale=-1.0),
                           reads=["rt", "cols"], writes=["rt"])
                      P.op("dve", lambda e: e.tensor_tensor(it[:], it[:], xc[:], ALU.mult), reads=["it", "xc"], writes=["it"])
                      P.op("dve", lambda e: e.tensor_tensor(it[:], it[:], rt[:], ALU.mult), reads=["it", "rt"], writes=["it"])
                      if d == 0:
                          prev = None
                          for (o0, n) in pieces:
                              init = 0.0 if prev is None else hf[:, o0 - 1:o0]
                              P.op("dve", lambda e, o0=o0, n=n, init=init: e.tensor_tensor_scan(
                                  hf[:, o0:o0 + n], at[:, o0:o0 + n], it[:, o0:o0 + n], init, ALU.mult, ALU.add),
                                   reads=["at", "it", "hf"], writes=["hf"])
                              prev = o0
                      else:
                          order = [(0, CT)] + [(CT + i * 512, 512) for i in reversed(range(8))]
                          first = True
                          for idx, (o0, n) in enumerate(order):
                              if idx == 0:
                                  init = 0.0
                              elif idx == 1:
                                  init = hb_[:, 0:1]
                              else:
                                  init = hb_[:, o0 + n:o0 + n + 1]
                              P.op("dve", lambda e, o0=o0, n=n, init=init: e.tensor_tensor_scan(
                                  hb_[:, o0:o0 + n][:, ::-1], at[:, o0:o0 + n][:, ::-1], it[:, o0:o0 + n][:, ::-1],
                                  init, ALU.mult, ALU.add), reads=["at", "it", "hb"], writes=["hb"])
                  P.op("dve", lambda e: e.tensor_tensor(hf[:, CT:L], hf[:, CT:L], hb_[:, CT:L], ALU.add),
                       reads=["hf", "hb"], writes=["hf"])
                  P.op("act", lambda e: e.activation(gbt[:], gbt[:], AF.Gelu_apprx_tanh), reads=["gbt"], writes=["gbt"])
                  P.op("dve", lambda e: e.tensor_tensor(lro[:], gbt[:], hf[:, CT:L], ALU.mult),
                       reads=["gbt", "hf"], writes=["lro"])
                  P.dma("sp", cat_d[0][512 + c * 128:512 + (c + 1) * 128, :], lro[:], reads=["lro"], writes=[f"cat0l{c}"])
              P.barrier()
          return phase_E(0, cat_d[0], wout_d, x_d, xmid_d if LAYERS > 1 else out_d)

    def phase_E(l, catd, w_d, xin_d, xout_d):
        final = []
        X = mybir.AxisListType.X
        with ExitStack() as esE:
            W12 = sb(esE, "W12", [128, 2, 32], F32)
            POSI = sb(esE, "POSI", [128, 2, 32], I32)
            WIDX = sb(esE, "WIDX", [128, 32], I32)
            WIDXD = sb(esE, "WIDXD", [128, 4, 32], I32)
            zt = sb(esE, "zt", [128, D], BF16)
            P.op("pool", lambda e: e.memset(zt[:], 0.0), writes=["zt"])
            for c8 in range(8):
                P.dma("pool", xs_d[c8 * 2048:(c8 + 1) * 2048, :].rearrange("(p j) d -> p j d", p=128),
                      zt[:].unsqueeze(1).to_broadcast([128, 16, D]), reads=["zt"], writes=[f"xs_zero{c8}"])
            with ExitStack() as es:
                H2B = sb(es, "H2B", [128, 32, D], BF16)
                SC = sb(es, "SC", [128, 32 * NE], F32)
                wo = sb(es, "wo", [128, 8, D], BF16)
                for k in range(8):
                    P.dma("pool", wo[:, k, :], w_d[k * 128:(k + 1) * 128, :], writes=["wo"])
                esr = ExitStack()
                rings = dict(
                    junk=Ring(esr, nc, "junkE", [128, D], BF16, 1),
                    stat=Ring(esr, nc, "statE", [128, 4], F32, 4),
                    t1=Ring(esr, nc, "t1E", [128, D], F32, 2),
                )
                catr = Ring(esr, nc, "catT", [128, 8, 512], BF16, 2)
                xr_ = Ring(esr, nc, "xtE", [128, D], F32, 2)
                tmr = Ring(esr, nc, "tmE", [128, D], F32, 2)
                x1r = Ring(esr, nc, "x1E", [128, D], F32, 2)
                h2r = Ring(esr, nc, "h2E", [128, D], F32, 3)
                h32r = Ring(esr, nc, "h32E", [128, 8, 128], F32, 2)
                mpr = Ring(esr, nc, "mixp", [128, 512], F32, 2, psum=True)
                tpr = Ring(esr, nc, "tpE", [128, 8, 128], F32, 1, psum=True)
                lgr = Ring(esr, nc, "lgE", [128, NE], F32, 2, psum=True)
                catv = catd.rearrange("(k p) t -> p k t", p=128)
                cat_state = {}
                tiles = {}
                tiles2 = {}

                def part1(t):
                    tcn, tt = t // 4, t % 4
                    if tt == 0:
                        catT, cres = catr.get()
                        P.dma("sp", catT[:], catv[:, :, tcn * 512:(tcn + 1) * 512], writes=[cres])
                        cat_state["cur"] = (catT, cres)
                    catT, cres = cat_state["cur"]
                    tok = t * 128
                    xt, xres = xr_.get()
                    P.dma("sp", xt[:], xin_d[tok:tok + 128, :], writes=[xres])
                    tm, tmres = tmr.get()
                    for half in range(2):
                        mp, mpres = mpr.get()
                        for k in range(8):
                            P.op("pe", lambda e, mp=mp, k=k, tt=tt, half=half, catT=catT: e.matmul(
                                mp[:], catT[:, k, tt * 128:(tt + 1) * 128], wo[:, k, half * 512:(half + 1) * 512],
                                start=(k == 0), stop=(k == 7)), reads=[cres, "wo"], writes=[mpres])
                        P.op("dve", lambda e, mp=mp, tm=tm, half=half: e.tensor_tensor(
                            tm[:, half * 512:(half + 1) * 512], mp[:], MOD[:, 2 * D + half * 512:2 * D + (half + 1) * 512],
                            ALU.mult), reads=[mpres, "MOD2"], writes=[tmres])
                    x1, x1res = x1r.get()
                    P.op("dve", lambda e, tm=tm, xt=xt, x1=x1: e.tensor_tensor(x1[:], tm[:], xt[:], ALU.add),
                         reads=[tmres, xres], writes=[x1res])
                    P.dma("act", x1_d[tok:tok + 128, :], x1[:], reads=[x1res], writes=[f"x1d_{t}"])
                    h2, h2res = h2r.get()
                    norm_mod(rings, x1[:], x1res, modsec("G2"), modsec("S2"), ["MOD3", "MOD4"], h2[:], h2res, "e", add_eng="dve")
                    P.op("act", lambda e, h2=h2, t=t: e.copy(H2B[:, t, :], h2[:]), reads=[h2res], writes=[f"H2B{t}"])
                    tiles[t] = (h2, h2res)

                def part2(t):
                    h2, h2res = tiles.pop(t)
                    tp, tpres = tpr.get()
                    for k in range(8):
                        P.op("pe", lambda e, tp=tp, h2=h2, k=k: e.transpose(tp[:, k, :], h2[:, k * 128:(k + 1) * 128],
                                                                            ident32[:]),
                             reads=[h2res, "ident32"], writes=[tpres])
                    h32, h32res = h32r.get()
                    P.op("act", lambda e, h32=h32, tp=tp: e.copy(h32[:], tp[:]), reads=[tpres], writes=[h32res])
                    tiles2[t] = (h32, h32res)

                def part2b(t):
                    h32, h32res = tiles2.pop(t)
                    lg, lgres = lgr.get()
                    for k in range(8):
                        P.op("pe", lambda e, lg=lg, h32=h32, k=k: e.matmul(lg[:], h32[:, k, :], rw32[:, k, :],
                                                                            start=(k == 0), stop=(k == 7)),
                             reads=[h32res, "rw32"], writes=[lgres])
                    P.op("dve", lambda e, lg=lg, t=t: e.tensor_copy(SC[:, t * NE:(t + 1) * NE], lg[:]),
                         reads=[lgres], writes=["route"])

                part1(0)
                part1(1)
                for t in range(32):
                    part2(t)
                    if t + 2 < 32:
                        part1(t + 2)
                    part2b(t)
                P.barrier()
                esr.close()
                RT = ["route"]

                def T512(name, dt=F32):
                    return sb(es, name, [128, 512], dt)
                SEL = T512("SEL"); W2 = T512("W2"); SELM = T512("SELM"); OH1 = T512("OH1"); OH2 = T512("OH2")
                POS = T512("POS"); PA = T512("PA"); PB = T512("PB"); TSB = T512("TSB"); Mb = T512("Mb", BF16)
                GM = sb(es, "GM", [128, 128], F32); GM2 = sb(es, "GM2", [128, 128], F32); GS = sb(es, "GS", [128, 128], F32)
                S32 = sb(es, "S32", [128, 8, 32], F32)
                S16 = sb(es, "S16", [128, 6, 16], F32)
                THR = sb(es, "THR", [128, 8], F32)
                CMPC = sb(es, "CMPC", [128, 16, 8], F32)
                PID = sb(es, "PID", [128, 8], F32)
                POSF = sb(es, "POSF", [128, 2, 32], F32)
                WF = sb(es, "WF", [128, 5, 32], F32)
                Lst = sb(es, "Lst", [128, 128], F32)
                Lsb = sb(es, "Lsb", [128, 128], BF16)
                Rps = psm(es, "Rps", [128, 512], F32)
                Tps = psm(es, "Tps", [128, 512], F32)

                def dv(fn, reads=RT, writes=RT):
                    P.op("dve", fn, reads=reads, writes=writes)
                v3 = lambda a: a[:].rearrange("p (a k) -> p a k", k=4)
                v16 = lambda a: a[:].rearrange("p (t e) -> p t e", e=16)
                g4 = lambda a: a[:].rearrange("p (t g) -> p t g", g=4)
                P.op("pool", lambda e: e.iota(S32[:, 7, :], [[1, 32]], base=0, channel_multiplier=0,
                                              allow_small_or_imprecise_dtypes=True), reads=RT, writes=RT)
                P.op("pool", lambda e: e.iota(THR[:], [[512, 8]], base=0, channel_multiplier=0,
                                              allow_small_or_imprecise_dtypes=True), reads=RT, writes=RT)
                for f in range(5):
                    P.op("pool", lambda e, f=f: e.iota(PID[:, f:f + 1], [[0, 1]], base=(128 * f + l * 8192) if f < 4 else l * 2048, channel_multiplier=1,
                                                       allow_small_or_imprecise_dtypes=True), reads=RT, writes=RT)
                P.op("pool", lambda e: e.memset(S16[:, 4, :], 1.0), reads=RT, writes=RT)
                P.op("pool", lambda e: e.memset(Lst[:], 1.0), reads=RT, writes=RT)
                P.op("pool", lambda e: e.affine_select(Lst[:], Lst[:], [[1, 128]], ALU.is_gt, 0.0, base=0,
                                                       channel_multiplier=-1), reads=RT, writes=RT)
                dv(lambda e: e.tensor_copy(Lsb[:], Lst[:]))
                P.op("act", lambda e: e.activation(SC[:], SC[:], AF.Sigmoid), reads=RT, writes=RT)
                dv(lambda e: e.tensor_tensor(v16(SEL), v16(SC), rbias[:].unsqueeze(1).to_broadcast([128, 32, NE]), ALU.add),
                   reads=RT + ["rbias"])
                dv(lambda e: e.tensor_reduce(GM[:], v3(SEL), X, ALU.max))
                dv(lambda e: e.tensor_tensor(v3(W2), v3(SEL), GM[:].unsqueeze(2).to_broadcast([128, 128, 4]), ALU.is_equal))
                dv(lambda e: e.scalar_tensor_tensor(W2[:], W2[:], -1000.0, SEL[:], ALU.mult, ALU.add))
                dv(lambda e: e.tensor_reduce(GM2[:], v3(W2), X, ALU.max))
                dv(lambda e: e.tensor_tensor(GS[:], GM[:], GM2[:], ALU.add))
                dv(lambda e: e.tensor_reduce(S32[:, 0, :], g4(GS), X, ALU.max))
                dv(lambda e: e.tensor_tensor(g4(GS), g4(GS), S32[:, 0, :].unsqueeze(2).to_broadcast([128, 32, 4]), ALU.is_equal))
                dv(lambda e: e.tensor_scalar(GS[:], GS[:], -1.0, 1000.0, ALU.add, ALU.mult))
                dv(lambda e: e.tensor_tensor(v3(SELM), v3(SEL), GS[:].unsqueeze(2).to_broadcast([128, 128, 4]), ALU.add))
                dv(lambda e: e.tensor_reduce(S32[:, 1, :], v16(SELM), X, ALU.max))
                dv(lambda e: e.tensor_tensor(v16(OH1), v16(SELM), S32[:, 1, :].unsqueeze(2).to_broadcast([128, 32, NE]), ALU.is_equal))
                dv(lambda e: e.scalar_tensor_tensor(W2[:], OH1[:], -1000.0, SELM[:], ALU.mult, ALU.add))
                dv(lambda e: e.tensor_reduce(S32[:, 2, :], v16(W2), X, ALU.max))
                dv(lambda e: e.tensor_tensor(v16(OH2), v16(W2), S32[:, 2, :].unsqueeze(2).to_broadcast([128, 32, NE]), ALU.is_equal))
                dv(lambda e: e.tensor_tensor(SEL[:], OH1[:], SC[:], ALU.mult))
                dv(lambda e: e.tensor_reduce(S32[:, 3, :], v16(SEL), X, ALU.add))
                dv(lambda e: e.tensor_tensor(SEL[:], OH2[:], SC[:], ALU.mult))
                dv(lambda e: e.tensor_reduce(S32[:, 4, :], v16(SEL), X, ALU.add))
                dv(lambda e: e.tensor_tensor(S32[:, 5, :], S32[:, 3, :], S32[:, 4, :], ALU.add))
                dv(lambda e: e.reciprocal(S32[:, 5, :], S32[:, 5, :]))
                dv(lambda e: e.tensor_tensor(W12[:, 0, :], S32[:, 3, :], S32[:, 5, :], ALU.mult), writes=RT + ["W12"])
                dv(lambda e: e.tensor_tensor(W12[:, 1, :], S32[:, 4, :], S32[:, 5, :], ALU.mult), writes=RT + ["W12"])
                dv(lambda e: e.tensor_tensor(Mb[:], OH1[:], OH2[:], ALU.add))
                P.op("pe", lambda e: e.matmul(Rps[:], Lsb[:], Mb[:], start=True, stop=True), reads=RT, writes=["Rps"])
                P.op("pe", lambda e: e.matmul(Tps[:], onesbf[:], Mb[:], start=True, stop=True), reads=RT + ["onesbf"], writes=["Tps"])
                dv(lambda e: e.tensor_copy(TSB[:], Tps[:]), reads=RT + ["Tps"])
                dv(lambda e: e.tensor_copy(PA[:], Tps[:]), reads=RT + ["Tps"])
                src, dst = PA, PB
                for s in (1, 2, 4, 8, 16):
                    w = s * NE
                    dv(lambda e, src=src, dst=dst, w=w: e.tensor_tensor(dst[:, w:512], src[:, w:512], src[:, 0:512 - w], ALU.add))
                    dv(lambda e, src=src, dst=dst, w=w: e.tensor_copy(dst[:, 0:w], src[:, 0:w]))
                    src, dst = dst, src
                PINC = src
                dv(lambda e: e.tensor_copy(S16[:, 0, :], PINC[:, 31 * NE:32 * NE]))
                dv(lambda e: e.tensor_tensor(CMPC[:], S16[:, 0, :].unsqueeze(2).to_broadcast([128, 16, 8]),
                                             THR[:].unsqueeze(1).to_broadcast([128, 16, 8]), ALU.is_gt))
                dv(lambda e: e.tensor_reduce(S16[:, 1, :], CMPC[:], X, ALU.add))
                dv(lambda e: e.tensor_tensor_scan(S16[:, 2, :], S16[:, 4, :], S16[:, 1, :], 0.0, ALU.mult, ALU.add))
                dv(lambda e: e.tensor_tensor(S16[:, 3, :], S16[:, 2, :], S16[:, 1, :], ALU.subtract))
                dv(lambda e: e.tensor_scalar(S16[:, 3, :], S16[:, 3, :], 512.0, None, ALU.mult))
                dv(lambda e: e.tensor_tensor(PINC[:], PINC[:], TSB[:], ALU.subtract))
                dv(lambda e: e.tensor_tensor(POS[:], Rps[:], PINC[:], ALU.add), reads=RT + ["Rps"])
                dv(lambda e: e.tensor_tensor(v16(POS), v16(POS), S16[:, 3, :].unsqueeze(1).to_broadcast([128, 32, NE]), ALU.add))
                dv(lambda e: e.tensor_tensor(SEL[:], OH1[:], POS[:], ALU.mult))
                dv(lambda e: e.tensor_reduce(POSF[:, 0, :], v16(SEL), X, ALU.add))
                dv(lambda e: e.tensor_tensor(SEL[:], OH2[:], POS[:], ALU.mult))
                dv(lambda e: e.tensor_reduce(POSF[:, 1, :], v16(SEL), X, ALU.add))
                dv(lambda e: e.tensor_copy(POSI[:], POSF[:]), writes=RT + ["POSI"])
                dv(lambda e: e.tensor_tensor(v16(SEL), S16[:, 2, :].unsqueeze(1).to_broadcast([128, 32, NE]),
                                             S32[:, 7, :].unsqueeze(2).to_broadcast([128, 32, NE]), ALU.is_le))
                dv(lambda e: e.tensor_reduce(S32[:, 6, :], v16(SEL), X, ALU.add))
                dv(lambda e: e.tensor_scalar(S32[:, 5, :], S32[:, 6, :], 16.0, 8192.0, ALU.is_ge, ALU.mult))
                dv(lambda e: e.tensor_scalar(S32[:, 6, :], S32[:, 6, :], 15.0, None, ALU.min))
                dv(lambda e: e.tensor_scalar(WF[:, 4, :], S32[:, 6, :], 128.0, PID[:, 4:5], ALU.mult, ALU.add))
                dv(lambda e: e.tensor_tensor(WF[:, 4, :], WF[:, 4, :], S32[:, 5, :], ALU.add))
                dv(lambda e: e.tensor_copy(WIDX[:], WF[:, 4, :]), writes=RT + ["WIDX"])
                for f in range(4):
                    dv(lambda e, f=f: e.tensor_scalar(WF[:, f, :], S32[:, 6, :], 512.0, PID[:, f:f + 1], ALU.mult, ALU.add))
                dv(lambda e: e.tensor_scalar(S32[:, 5, :], S32[:, 5, :], 4.0, None, ALU.mult))
                for f in range(4):
                    dv(lambda e, f=f: e.tensor_tensor(WF[:, f, :], WF[:, f, :], S32[:, 5, :], ALU.add))
                dv(lambda e: e.tensor_copy(WIDXD[:], WF[:, 0:4, :]), writes=RT + ["WIDX"])
                for t in range(32):
                    for k in range(2):
                        P.dma("pool", xs_d[:, :], H2B[:, t, :], ind=(POSI[:, k, t:t + 1], None),
                              reads=["POSI", f"H2B{t}"] + [f"xs_zero{c8}" for c8 in range(8)], writes=[f"xs_{t}_{k}"],
                              bounds_check=16383, oob_is_err=False)
                P.barrier()
            with ExitStack() as es:
                xsr = Ring(es, nc, "xsb", [128, 4, D], BF16, 2)
                xTr = Ring(es, nc, "xTm", [128, 8, 512], BF16, 2)
                wgr = Ring(es, nc, "wg", [128, 8 * DFF], BF16, 2)
                wur = Ring(es, nc, "wu", [128, 8 * DFF], BF16, 2)
                wdr = Ring(es, nc, "wd", [128, 4, D], BF16, 2)
                pTr = Ring(es, nc, "pTm", [128, D], BF16, 2, psum=True)
                pgr = Ring(es, nc, "pg", [128, 512], F32, 2, psum=True)
                pur = Ring(es, nc, "pu", [128, 512], F32, 2, psum=True)
                pyr = Ring(es, nc, "py", [128, 512], F32, 2, psum=True)
                sgr = Ring(es, nc, "sg", [128, 512], F32, 3)
                acr = Ring(es, nc, "acT", [128, 4, 512], BF16, 2)
                ysr = Ring(es, nc, "ys", [128, D], F32, 3)
                loaded = {}

                def issue_loads(i):
                    xsb, xsres = xsr.get()
                    P.dma("sp", xsb[:], xs_d[i * 512:(i + 1) * 512, :].rearrange("(j p) d -> p j d", p=128), writes=[xsres])
                    wg, wgres = wgr.get()
                    wu, wures = wur.get()
                    wd, wdres = wdr.get()
                    P.dma("pool", wg[:, :], wg_d[:, :], ind=(None, WIDX[:, i:i + 1]),
                          reads=["WIDX"], writes=[wgres], bounds_check=4095, oob_is_err=False)
                    P.dma("pool", wu[:, :], wu_d[:, :], ind=(None, WIDX[:, i:i + 1]),
                          reads=["WIDX"], writes=[wures], bounds_check=4095, oob_is_err=False)
                    for f in range(4):
                        P.dma("pool", wd[:, f, :], wd_d[:, :], ind=(None, WIDXD[:, f, i:i + 1]), reads=["WIDX"], writes=[wdres],
                              bounds_check=16383, oob_is_err=False)
                    loaded[i] = (xsb, xsres, wg, wgres, wu, wures, wd, wdres)

                xts = {}

                def do_transposes(i):
                    xsb, xsres = loaded[i][0], loaded[i][1]
                    xT, xTres = xTr.get()
                    for j in range(4):
                        pt, ptres = pTr.get()
                        for k in range(8):
                            P.op("pe", lambda e, pt=pt, xsb=xsb, j=j, k=k: e.transpose(
                                pt[:, k * 128:(k + 1) * 128], xsb[:, j, k:D:8], identbf[:]),
                                 reads=[xsres, "identbf"], writes=[ptres])
                        if j % 2 == 0:
                            P.op("act", lambda e, pt=pt, xT=xT, j=j: e.copy(xT[:, :, j * 128:(j + 1) * 128],
                                                                              pt[:].rearrange("p (k t) -> p k t", k=8)),
                                 reads=[ptres], writes=[xTres])
                        else:
                            P.op("dve", lambda e, pt=pt, xT=xT, j=j: e.tensor_copy(xT[:, :, j * 128:(j + 1) * 128],
                                                                                    pt[:].rearrange("p (k t) -> p k t", k=8)),
                                 reads=[ptres], writes=[xTres])
                    xts[i] = (xT, xTres)

                issue_loads(0)
                issue_loads(1)
                do_transposes(0)
                for i in range(32):
                    xsb, xsres, wg, wgres, wu, wures, wd, wdres = loaded[i]
                    xT, xTres = xts.pop(i)
                    for f in range(4):
                        P.op("dve", lambda e, wd=wd, f=f: e.tensor_tensor(wd[:, f, :], wd[:, f, :], modsec("GATE2"), ALU.mult),
                             reads=[wdres, "MOD5"], writes=[wdres])
                    acT, acres = acr.get()
                    for f in range(4):
                        pg, pgres = pgr.get()
                        pu, pures = pur.get()
                        for k in range(8):
                            P.op("pe", lambda e, pg=pg, wg=wg, xT=xT, k=k, f=f: e.matmul(
                                pg[:], wg[:, k * DFF + f * 128:k * DFF + (f + 1) * 128], xT[:, k, :], start=(k == 0), stop=(k == 7)),
                                 reads=[wgres, xTres], writes=[pgres])
                        for k in range(8):
                            P.op("pe", lambda e, pu=pu, wu=wu, xT=xT, k=k, f=f: e.matmul(
                                pu[:], wu[:, k * DFF + f * 128:k * DFF + (f + 1) * 128], xT[:, k, :], start=(k == 0), stop=(k == 7)),
                                 reads=[wures, xTres], writes=[pures])
                        sg, sgres = sgr.get()
                        P.op("act", lambda e, sg=sg, pg=pg: e.activation(sg[:], pg[:], AF.Silu), reads=[pgres], writes=[sgres])
                        P.op("dve", lambda e, acT=acT, pu=pu, sg=sg, f=f: e.tensor_tensor(acT[:, f, :], pu[:], sg[:], ALU.mult),
                             reads=[pures, sgres], writes=[acres])
                    if i + 1 < 32:
                        do_transposes(i + 1)
                    for jj in range(4):
                        ys, ysres = ysr.get()
                        for half in range(2):
                            py, pyres = pyr.get()
                            for f in range(4):
                                P.op("pe", lambda e, py=py, acT=acT, wd=wd, f=f, jj=jj, half=half: e.matmul(
                                    py[:], acT[:, f, jj * 128:(jj + 1) * 128], wd[:, f, half * 512:(half + 1) * 512],
                                    start=(f == 0), stop=(f == 3)), reads=[acres, wdres], writes=[pyres])
                            if half == 0:
                                P.op("act", lambda e, ys=ys, py=py: e.copy(ys[:, 0:512], py[:]), reads=[pyres], writes=[ysres])
                            else:
                                P.op("dve", lambda e, ys=ys, py=py: e.tensor_copy(ys[:, 512:D], py[:]), reads=[pyres], writes=[ysres])
                        r0 = i * 512 + jj * 128
                        P.dma("act", y_d[r0:r0 + 128, :], ys[:], reads=[ysres], writes=[f"yd_{i}_{jj}"])
                    loaded.pop(i)
                    if i + 2 < 32:
                        issue_loads(i + 2)
                P.barrier()
                xcr = Ring(es, nc, "xcmb", [128, D], F32, 4)
                y1r = Ring(es, nc, "y1c", [128, D], F32, 3)
                y2r = Ring(es, nc, "y2c", [128, D], F32, 3)
                for t in range(32):
                    tok = t * 128
                    xt, xres = xcr.get()
                    P.dma("sp", xt[:], x1_d[tok:tok + 128, :], writes=[xres])
                    y1, y1res = y1r.get()
                    y2, y2res = y2r.get()
                    P.dma("pool", y1[:, :], y_d[:, :], ind=(None, POSI[:, 0, t:t + 1]), reads=["POSI"], writes=[y1res],
                          bounds_check=16383, oob_is_err=False)
                    P.dma("pool", y2[:, :], y_d[:, :], ind=(None, POSI[:, 1, t:t + 1]), reads=["POSI"], writes=[y2res],
                          bounds_check=16383, oob_is_err=False)
                    P.op("dve", lambda e, xt=xt, y1=y1, t=t: e.scalar_tensor_tensor(xt[:], y1[:], W12[:, 0, t:t + 1], xt[:],
                                                                                    ALU.mult, ALU.add),
                         reads=[xres, y1res, "W12"], writes=[xres])
                    P.op("dve", lambda e, xt=xt, y2=y2, t=t: e.scalar_tensor_tensor(xt[:], y2[:], W12[:, 1, t:t + 1], xt[:],
                                                                                    ALU.mult, ALU.add),
                         reads=[xres, y2res, "W12"], writes=[xres])
                    oid = P.dma("act", xout_d[tok:tok + 128, :], xt[:], reads=[xres], writes=[f"xout_{t}"])
                    final.append(oid)
                P.barrier()
        return final

    def layer1():
        phase_mods(1)
        with ExitStack() as esF:
            Ac = sb(esF, "Ac", [128, NT, D], BF16)
            As = sb(esF, "As", [128, NT, D], BF16)
            with ExitStack() as es:
                cc = sb(es, "cc", [128, 2, 2, 256], BF16)
                P.dma("sp", cc[:], cc_d, writes=["cc"])
                rings = dict(
                    junk=Ring(es, nc, "junkF", [128, D], BF16, 1),
                    stat=Ring(es, nc, "statF", [128, 4], F32, 4),
                    t1=Ring(es, nc, "t1F", [128, D], F32, 2),
                    pT=Ring(es, nc, "pTF", [128, D], BF16, 2, psum=True),
                )
                xr_ = Ring(es, nc, "xtF", [128, D], F32, 3)
                hbr = Ring(es, nc, "hbfF", [128, D], BF16, 4)
                hTr = Ring(es, nc, "hTF", [128, 8, 128], BF16, 3)
                par = Ring(es, nc, "paF", [128, 2, 512], F32, 3, psum=True)
                st1 = {}
                st2 = {}

                def f_stage1(t):
                    xt, xres = xr_.get()
                    P.dma("sp", xt[:], xmid_d[t * 128:(t + 1) * 128, :], writes=[xres])
                    hb, hbres = hbr.get()
                    norm_mod(rings, xt[:], xres, modsec("G1"), modsec("S1"), ["MOD0", "MOD1"], hb[:], hbres, "f", add_eng="dve")
                    st1[t] = (hb, hbres)

                def f_stage2(t):
                    hb, hbres = st1.pop(t)
                    hT, hTres = hTr.get()
                    transpose_tile(rings, hb, hbres, hT[:], hTres, identbf, BF16)
                    st2[t] = (hT, hTres)

                def f_stage3(t):
                    hT, hTres = st2.pop(t)
                    for cs in range(2):
                        pa, pares = par.get()
                        for g in range(4):
                            for kk in range(2):
                                P.op("pe", lambda e, pa=pa, hT=hT, cs=cs, g=g, kk=kk: e.matmul(
                                    pa[:, g // 2, (g % 2) * 256:(g % 2 + 1) * 256], hT[:, 2 * g + kk, :], cc[:, kk, cs, :],
                                    start=(kk == 0), stop=(kk == 1)), reads=[hTres, "cc"], writes=[pares])
                        A = Ac if cs == 0 else As
                        an = "Ac" if cs == 0 else "As"
                        P.op("act", lambda e, pa=pa, t=t, A=A: e.copy(A[:, t, 0:512], pa[:, 0, :]), reads=[pares], writes=[an])
                        P.op("dve", lambda e, pa=pa, t=t, A=A: e.tensor_copy(A[:, t, 512:D], pa[:, 1, :]), reads=[pares], writes=[an])

                f_stage1(0)
                f_stage1(1)
                f_stage2(0)
                for t in range(NT):
                    if t + 2 < NT:
                        f_stage1(t + 2)
                    if t + 1 < NT:
                        f_stage2(t + 1)
                    f_stage3(t)
                P.barrier()
            with ExitStack() as es:
                dfr = Ring(es, nc, "dfp", [128, 2, 512], BF16, 4)
                pbr = Ring(es, nc, "pbF", [128, 512], F32, 8, psum=True)
                fsr = Ring(es, nc, "fst", [128, 512], BF16, 4)
                qsr = Ring(es, nc, "qsF", [128, 512], F32, 2)
                for g8 in range(4):
                    for nq in range(4):
                        Pb = [pbr.get() for _ in range(2)]
                        Qb = [pbr.get() for _ in range(2)]
                        for nk in range(NT):
                            df, dfres = dfr.get()
                            P.dma("sp", df[:], dftn_d[nq, nk], writes=[dfres])
                            for f2 in range(2):
                                col = g8 * 256 + f2 * 128
                                P.op("pe", lambda e, pb=Pb[f2][0], nk=nk, col=col, df=df: e.matmul(
                                    pb[:], Ac[:, nk, col:col + 128], df[:, 0, :], start=(nk == 0), stop=(nk == NT - 1)),
                                     reads=[dfres, "Ac"], writes=[Pb[f2][1]])
                                P.op("pe", lambda e, qb=Qb[f2][0], nk=nk, col=col, df=df: e.matmul(
                                    qb[:], As[:, nk, col:col + 128], df[:, 1, :], start=(nk == 0), stop=(nk == NT - 1)),
                                     reads=[dfres, "As"], writes=[Qb[f2][1]])
                        for f2 in range(2):
                            row = g8 * 256 + f2 * 128
                            qs, qsres = qsr.get()
                            P.op("act", lambda e, qs=qs, qb=Qb[f2][0]: e.copy(qs[:], qb[:]), reads=[Qb[f2][1]], writes=[qsres])
                            fa, fares = fsr.get()
                            P.op("dve", lambda e, fa=fa, pb=Pb[f2][0], qs=qs: e.tensor_tensor(fa[:], pb[:], qs[:], ALU.add),
                                 reads=[Pb[f2][1], qsres], writes=[fares])
                            P.dma("act", cat_d[1][row:row + 128, 1 + nq * 512:1 + (nq + 1) * 512], fa[:], reads=[fares],
                                  writes=[f"cat1a_{g8}_{nq}_{f2}"])
                            fb, fbres = fsr.get()
                            P.op("dve", lambda e, fb=fb, pb=Pb[f2][0], qs=qs: e.tensor_tensor(fb[:, ::-1], pb[:], qs[:], ALU.subtract),
                                 reads=[Pb[f2][1], qsres], writes=[fbres])
                            c0 = 3584 - 512 * nq
                            P.dma("act", cat_d[1][row:row + 128, c0:c0 + 512], fb[:], reads=[fbres],
                                  writes=[f"cat1b_{g8}_{nq}_{f2}"])
                p0, p0res = pbr.get()
                for k in range(8):
                    for nk in range(NT):
                        P.op("pe", lambda e, k=k, nk=nk: e.matmul(p0[:, k:k + 1], Ac[:, nk, k * 128:(k + 1) * 128], onesbf[:, 0:1],
                                                                   start=(nk == 0), stop=(nk == NT - 1)),
                             reads=["Ac", "onesbf"], writes=[p0res])
                f0 = sb(es, "f0col", [128, 8], BF16)
                P.op("dve", lambda e: e.tensor_copy(f0[:], p0[:, 0:8]), reads=[p0res], writes=["f0col"])
                P.dma("sp", cat_d[1][:, 0:1].rearrange("(k p) o -> p (k o)", p=128), f0[:], reads=["f0col"], writes=["cat1_col0"],
                      allow_slow_non_contiguous=True)
                P.barrier()
        return phase_E(1, cat_d[1], fw_d, xmid_d, out_d)

    fin = layer0()
    if LAYERS > 1:
        fin = layer1()
    P.emit(final_wait_ops=fin)
    top.close()
    return nc, P


_CONST = {}


def _consts():
    if _CONST:
        return _CONST
    bf = ml_dtypes.bfloat16
    p = np.arange(128)
    cp = np.arange(256)
    cc = np.zeros((128, 2, 2, 256), np.float64)
    for kk in range(2):
        ang = 2 * np.pi * (((kk * 128 + p)[:, None] * cp[None, :]) % 256) / 256.0
        cc[:, kk, 0, :] = np.cos(ang) / 1024.0
        cc[:, kk, 1, :] = np.sin(ang) / 1024.0
    _CONST["dft_c"] = cc.astype(np.float32).astype(bf)
    n = np.arange(N, dtype=np.int64)
    m = (n[:, None] * n[None, :]) % N
    tab = np.cos(2 * np.pi * np.arange(N) / N).astype(np.float32)
    tabs = (-np.sin(2 * np.pi * np.arange(N) / N)).astype(np.float32)
    Cn = tab[m].astype(bf)
    Sn = tabs[m].astype(bf)
    arr = np.empty((4, 32, 128, 2, 512), bf)
    Cr = Cn[:, 1:2049].reshape(32, 128, 4, 512)
    Sr = Sn[:, 1:2049].reshape(32, 128, 4, 512)
    arr[:, :, :, 0, :] = Cr.transpose(2, 0, 1, 3)
    arr[:, :, :, 1, :] = Sr.transpose(2, 0, 1, 3)
    _CONST["dft_n"] = np.ascontiguousarray(arr)
    return _CONST


def _layout_inputs(inp):
    f32 = np.float32
    shared = {}
    shared["c_ctx"] = np.ascontiguousarray(inp["c_ctx"].reshape(8, 128), f32)
    for k in ("ada_w", "ada_b", "norm_mix", "norm_ffn", "router_w"):
        shared[k] = np.ascontiguousarray(inp[k], f32)
    shared["moe_w_gate"] = np.ascontiguousarray(inp["moe_w_gate"], f32).reshape(2 * NE * 128, 8 * DFF)
    shared["moe_w_up"] = np.ascontiguousarray(inp["moe_w_up"], f32).reshape(2 * NE * 128, 8 * DFF)
    shared["moe_w_down"] = np.ascontiguousarray(inp["moe_w_down"], f32).reshape(2 * NE * DFF, D)
    shared["mix_w_in"] = np.ascontiguousarray(inp["mix_w_in"][0], f32)
    shared["mix_w_out"] = np.ascontiguousarray(inp["mix_w_out"][0], f32)
    shared["fnet_w_out"] = np.ascontiguousarray(inp["fnet_w_out"][0], f32)
    shared["router_bias"] = np.ascontiguousarray(inp["router_bias"].reshape(1, NE), f32)
    qk = np.stack([np.tile(inp["na_q_norm"][0], 2), np.tile(inp["na_k_norm"][0], 2)], axis=1)
    shared["qk_gain"] = np.ascontiguousarray(qk, f32)
    rpb = np.asarray(inp["na_rpb"][0], f32)
    kc = np.arange(64)
    cq = np.arange(64)
    cs = np.clip(cq - 8, 0, 48)
    valid = (kc[:, None] >= cs[None, :]) & (kc[:, None] < cs[None, :] + 16)
    dc = np.clip(kc[:, None] - cq[None, :], -15, 15) + 15
    bt = np.empty((4, 128, 2, 14, 64), f32)
    for pr in range(4):
        for h in range(2):
            for half in range(2):
                for ds in range(14):
                    g = rpb[2 * pr + h, ds + half][dc]
                    bt[pr, half * 64:(half + 1) * 64, h, ds, :] = np.where(valid, g, f32(MASKV))
    shared["bt"] = bt
    cw = np.asarray(inp["lru_conv_w"][0], f32)
    shared["lru_cw"] = np.ascontiguousarray(cw.reshape(4, 4, 128).transpose(2, 1, 0))
    shared["lru_cb"] = np.ascontiguousarray(np.asarray(inp["lru_conv_b"][0], f32).reshape(4, 128).T)
    wbd = np.zeros((128, 16, 128), f32)
    gbias = np.empty((128, 16), f32)
    for gi, (wk, bk) in enumerate((("lru_gate_r_w", "lru_gate_r_b"), ("lru_gate_i_w", "lru_gate_i_b"))):
        w = np.asarray(inp[wk][0], f32)
        b = np.asarray(inp[bk][0], f32)
        for d in range(2):
            for c in range(4):
                idx = (gi * 2 + d) * 4 + c
                wbd[0:64, idx, 0:64] = w[d, 2 * c]
                wbd[64:128, idx, 64:128] = w[d, 2 * c + 1]
                gbias[:, idx] = b[d, c * 128:(c + 1) * 128]
    shared["lru_wbd"] = wbd
    shared["lru_gbias"] = gbias
    lam = np.asarray(inp["lru_lambda"][0], f32)
    shared["lru_lam"] = np.ascontiguousarray(lam.reshape(2, 4, 128).transpose(2, 0, 1).reshape(128, 8))
    shared.update(_consts())
    maps = []
    for b in range(8):
        m = dict(shared)
        m["x"] = np.ascontiguousarray(inp["x"][b], f32)
        m["c"] = np.ascontiguousarray(inp["c"][b].reshape(8, 128), f32)
        m["ctx"] = np.ascontiguousarray(inp["ctx"][b], f32)
        maps.append(m)
    return maps


_PROG = {}


def kernel(**inputs):
    inp = {k: np.asarray(v) for k, v in inputs.items()}
    maps = _layout_inputs(inp)
    if "nc" not in _PROG:
        _PROG["nc"], _PROG["P"] = build_program()
    nc = _PROG["nc"]
    ncores = int(os.environ.get("MK_CORES", "8"))
    res = run_bass_kernel_spmd(nc, maps[:ncores], core_ids=list(range(ncores)))
    if DEBUG:
        _PROG["res"] = res
    out = np.zeros((8, N, D), np.float32)
    for b in range(ncores):
        out[b] = res.results[b]["out"]
    return out
```
